# Optimizing a Trainium2 kernel written in Bass

```python
import math
import jax, jax.numpy as jnp
from jax import lax
import numpy as np

D_MODEL = 4096
BATCH = 2
SEQ = 8192
DEPTH = 1

RET_HEADS = 16
RET_DK = 128
RET_DV = 256
RET_CHUNK = 128
RET_QK = RET_HEADS * RET_DK
RET_V = RET_HEADS * RET_DV
SWA_Q_HEADS = 64
SWA_KV_HEADS = 8
SWA_HEAD_DIM = 64
SWA_WINDOW = 128
SWA_BLOCK = 128
SWA_Q = SWA_Q_HEADS * SWA_HEAD_DIM
SWA_KV = SWA_KV_HEADS * SWA_HEAD_DIM
MEM_LEN = 256
XATTN_HEADS = 4
XATTN_HEAD_DIM = 256
XATTN_W = XATTN_HEADS * XATTN_HEAD_DIM
N_GROUPS = 8
EXPERTS_PER_GROUP = 8
N_EXPERTS = N_GROUPS * EXPERTS_PER_GROUP
TOP_K = 2
D_EXPERT = 512
MOE_BLOCK = 128
RMS_EPS = 1e-6
ROPE_BASE = 10000.0
NEG_INF = -1e30

IN_WIDTHS = [RET_QK, RET_QK, RET_V, RET_V, SWA_Q, SWA_KV, SWA_KV, D_MODEL, D_MODEL]
IN_WIDTH = sum(IN_WIDTHS)
IN_SPLITS = [int(v) for v in np.cumsum(IN_WIDTHS)[:-1]]

kernel_name = "hybrid_retention_swa_sink_hmoe_block"


def rms_norm(x, g):
    xf = x.astype(jnp.float32)
    y = xf * lax.rsqrt(jnp.mean(xf * xf, axis=-1, keepdims=True) + RMS_EPS)
    return (y * g.astype(jnp.float32)).astype(x.dtype)


def rotary(x, pos):
    half = x.shape[-1] // 2
    inv = ROPE_BASE ** (-jnp.arange(half, dtype=jnp.float32) / half)
    ang = pos.astype(jnp.float32)[:, None] * inv[None, :]
    cos = jnp.cos(ang)[None, :, None, :]
    sin = jnp.sin(ang)[None, :, None, :]
    x1, x2 = x[..., :half], x[..., half:]
    return jnp.concatenate([x1 * cos - x2 * sin, x1 * sin + x2 * cos], axis=-1)


def retention_chunkwise(q, k, v, log_gamma):
    B, S, H, dk = q.shape
    dv = v.shape[-1]
    C = RET_CHUNK
    N = S // C
    qc = q.reshape(B, N, C, H, dk)
    kc = k.reshape(B, N, C, H, dk)
    vc = v.reshape(B, N, C, H, dv)
    idx = jnp.arange(C, dtype=jnp.float32)
    diff = idx[:, None] - idx[None, :]
    decay = jnp.where(diff[None] >= 0,
                      jnp.exp(log_gamma[:, None, None] * jnp.maximum(diff, 0.0)[None]), 0.0)
    scores = jnp.einsum('bnihd,bnjhd->bnhij', qc, kc) * decay[None, None]
    intra = jnp.einsum('bnhij,bnjhv->bnihv', scores, vc)
    q_dec = jnp.exp(log_gamma[:, None] * (idx + 1.0)[None, :]).T[None, :, :, None]
    k_dec = jnp.exp(log_gamma[:, None] * (C - 1.0 - idx)[None, :]).T[None, :, :, None]
    chunk_dec = jnp.exp(log_gamma * C)[None, :, None, None]

    def step(state, inp):
        qi, ki, vi = inp
        cross = jnp.einsum('bihd,bhdv->bihv', qi * q_dec, state)
        new_state = state * chunk_dec + jnp.einsum('bjhd,bjhv->bhdv', ki * k_dec, vi)
        return new_state, cross

    init = jnp.zeros((B, H, dk, dv), jnp.float32)
    _, cross = lax.scan(step, init, (jnp.moveaxis(qc, 1, 0), jnp.moveaxis(kc, 1, 0), jnp.moveaxis(vc, 1, 0)))
    out = intra + jnp.moveaxis(cross, 0, 1)
    return out.reshape(B, S, H, dv)


def sliding_window_gqa_sinks(q, k, v, sinks):
    B, S, Hq, d = q.shape
    Hkv = k.shape[2]
    G = Hq // Hkv
    C = SWA_BLOCK
    N = S // C
    scale = 1.0 / math.sqrt(d)
    qb = jnp.moveaxis(q.reshape(B, N, C, Hkv, G, d), 1, 0)

    def band(t):
        tb = t.reshape(B, N, C, Hkv, d)
        prev = jnp.concatenate([jnp.zeros_like(tb[:, :1]), tb[:, :-1]], axis=1)
        return jnp.moveaxis(jnp.concatenate([prev, tb], axis=2), 1, 0)

    kband, vband = band(k), band(v)
    qi = jnp.arange(C)
    kj = jnp.arange(2 * C)
    rel = C + qi[:, None] - kj[None, :]
    in_window = (rel >= 0) & (rel < SWA_WINDOW)
    sink = sinks.astype(jnp.float32).reshape(Hkv, G)[None, :, :, None]

    def block(inp):
        n, qn, kn, vn = inp
        s = jnp.einsum('bihgd,bjhd->bhgij', qn, kn).astype(jnp.float32) * scale
        valid = in_window & ((n * C - C + kj) >= 0)[None, :]
        s = jnp.where(valid, s, NEG_INF)
        m = jnp.maximum(jnp.max(s, axis=-1), sink)
        p = jnp.exp(s - m[..., None])
        denom = jnp.sum(p, axis=-1) + jnp.exp(sink - m)
        o = jnp.einsum('bhgij,bjhd->bihgd', p / denom[..., None], vn.astype(jnp.float32))
        return o.astype(qn.dtype)

    out = lax.map(block, (jnp.arange(N), qb, kband, vband))
    return jnp.moveaxis(out, 0, 1).reshape(B, S, Hq * d)


def memory_cross_attention(h, m, w_xq, w_xkv, w_xo):
    B, S, _ = h.shape
    q = (h @ w_xq).reshape(B, S, XATTN_HEADS, XATTN_HEAD_DIM)
    k, v = jnp.split(m @ w_xkv, 2, axis=-1)
    k = k.reshape(B, -1, XATTN_HEADS, XATTN_HEAD_DIM)
    v = v.reshape(B, -1, XATTN_HEADS, XATTN_HEAD_DIM)
    s = jnp.einsum('bshd,bmhd->bhsm', q, k).astype(jnp.float32) / math.sqrt(XATTN_HEAD_DIM)
    p = jax.nn.softmax(s, axis=-1).astype(v.dtype)
    o = jnp.einsum('bhsm,bmhd->bshd', p, v).reshape(B, S, XATTN_W)
    return o @ w_xo


def hierarchical_moe(h, w_rg, b_rg, w_re, b_re, w_gate, w_up, w_down):
    B, S, D = h.shape
    T = B * S
    xt = h.reshape(T, D)
    g_prob = jax.nn.softmax((xt @ w_rg).astype(jnp.float32) + b_rg.astype(jnp.float32), axis=-1)
    g_top, g_idx = lax.top_k(g_prob, 1)
    e_logits = ((xt @ w_re).astype(jnp.float32) + b_re.astype(jnp.float32)).reshape(T, N_GROUPS, EXPERTS_PER_GROUP)
    in_group = jnp.take_along_axis(e_logits, g_idx[:, :, None], axis=1)[:, 0]
    top_logit, top_local = lax.top_k(in_group, TOP_K)
    top_w = jax.nn.softmax(top_logit, axis=-1) * g_top
    expert = g_idx * EXPERTS_PER_GROUP + top_local

    A = T * TOP_K
    e_flat = expert.reshape(A)
    tok_flat = jnp.repeat(jnp.arange(T, dtype=jnp.int32), TOP_K)
    order = jnp.argsort(e_flat)
    e_sorted = e_flat[order]
    counts = jnp.zeros((N_EXPERTS,), jnp.int32).at[e_flat].add(1)
    padded = ((counts + MOE_BLOCK - 1) // MOE_BLOCK) * MOE_BLOCK
    start = jnp.cumsum(counts) - counts
    pend = jnp.cumsum(padded)
    pstart = pend - padded
    ppos_sorted = pstart[e_sorted] + jnp.arange(A, dtype=jnp.int32) - start[e_sorted]
    P = A + N_EXPERTS * MOE_BLOCK
    nb = P // MOE_BLOCK
    buf_tok = jnp.full((P,), T, jnp.int32).at[ppos_sorted].set(tok_flat[order])
    block_expert = jnp.minimum(
        jnp.searchsorted(pend, jnp.arange(nb, dtype=jnp.int32) * MOE_BLOCK, side='right'), N_EXPERTS - 1)
    x_pad = jnp.concatenate([xt, jnp.zeros((1, D), xt.dtype)], axis=0)

    def run_block(inp):
        tok, e = inp
        xb = x_pad[tok]
        return (jax.nn.silu(xb @ w_gate[e]) * (xb @ w_up[e])) @ w_down[e]

    out = lax.map(run_block, (buf_tok.reshape(nb, MOE_BLOCK), block_expert)).reshape(P, D)
    assign_pos = jnp.zeros((A,), jnp.int32).at[order].set(ppos_sorted)
    y = jnp.einsum('tk,tkd->td', top_w.astype(out.dtype), out[assign_pos].reshape(T, TOP_K, D))
    return y.reshape(B, S, D)


def setup_inputs(seed: int = 0) -> dict:
    key = jax.random.key(seed)
    ks = jax.random.split(key, 24)
    f32 = jnp.float32

    def nrm(k, shape, fan_in, mult=1.0):
        return jax.random.normal(k, shape, f32) * (mult * fan_in ** -0.5)

    def gain(k, shape):
        return 1.0 + 0.02 * jax.random.normal(k, shape, f32)

    return {
        "x": jax.random.normal(ks[0], (BATCH, SEQ, D_MODEL), f32),
        "mem": jax.random.normal(ks[1], (BATCH, MEM_LEN, D_MODEL), f32),
        "mix_norm_g": gain(ks[2], (DEPTH, D_MODEL)),
        "w_in": nrm(ks[3], (DEPTH, D_MODEL, IN_WIDTH), D_MODEL),
        "ret_norm_g": gain(ks[4], (DEPTH, RET_HEADS, RET_DV)),
        "swa_sinks": 0.5 * jax.random.normal(ks[5], (DEPTH, SWA_Q_HEADS), f32),
        "w_ret_o": nrm(ks[6], (DEPTH, RET_V, D_MODEL), RET_V),
        "w_swa_o": nrm(ks[7], (DEPTH, SWA_Q, D_MODEL), SWA_Q),
        "w_mix_o": nrm(ks[8], (DEPTH, D_MODEL, D_MODEL), D_MODEL),
        "xattn_norm_g": gain(ks[9], (DEPTH, D_MODEL)),
        "mem_norm_g": gain(ks[10], (DEPTH, D_MODEL)),
        "w_xq": nrm(ks[11], (DEPTH, D_MODEL, XATTN_W), D_MODEL),
        "w_xkv": nrm(ks[12], (DEPTH, D_MODEL, 2 * XATTN_W), D_MODEL),
        "w_xo": nrm(ks[13], (DEPTH, XATTN_W, D_MODEL), XATTN_W),
        "moe_norm_g": gain(ks[14], (DEPTH, D_MODEL)),
        "w_router_group": nrm(ks[15], (DEPTH, D_MODEL, N_GROUPS), D_MODEL),
        "b_router_group": 0.01 * jax.random.normal(ks[16], (DEPTH, N_GROUPS), f32),
        "w_router_expert": nrm(ks[17], (DEPTH, D_MODEL, N_EXPERTS), D_MODEL),
        "b_router_expert": 0.01 * jax.random.normal(ks[18], (DEPTH, N_EXPERTS), f32),
        "w_exp_gate": nrm(ks[19], (DEPTH, N_EXPERTS, D_MODEL, D_EXPERT), D_MODEL),
        "w_exp_up": nrm(ks[20], (DEPTH, N_EXPERTS, D_MODEL, D_EXPERT), D_MODEL),
        "w_exp_down": nrm(ks[21], (DEPTH, N_EXPERTS, D_EXPERT, D_MODEL), D_EXPERT),
        "final_norm_g": gain(ks[22], (D_MODEL,)),
    }


def reference(x, mem, mix_norm_g, w_in, ret_norm_g, swa_sinks, w_ret_o, w_swa_o, w_mix_o,
              xattn_norm_g, mem_norm_g, w_xq, w_xkv, w_xo, moe_norm_g,
              w_router_group, b_router_group, w_router_expert, b_router_expert,
              w_exp_gate, w_exp_up, w_exp_down, final_norm_g):
    B, S, _ = x.shape
    pos = jnp.arange(S, dtype=jnp.int32)
    log_gamma = jnp.log1p(-jnp.exp2(-5.0 - jnp.arange(RET_HEADS, dtype=jnp.float32)))
    for l in range(DEPTH):
        h = rms_norm(x, mix_norm_g[l])
        proj = h @ w_in[l]
        rq, rk, rv, rg, sq, sk, sv, a_ret, a_swa = jnp.split(proj, IN_SPLITS, axis=-1)
        q_r = rotary(rq.reshape(B, S, RET_HEADS, RET_DK).astype(jnp.float32), pos)
        k_r = rotary(rk.reshape(B, S, RET_HEADS, RET_DK).astype(jnp.float32), pos) * (RET_DK ** -0.5)
        v_r = rv.reshape(B, S, RET_HEADS, RET_DV).astype(jnp.float32)
        ret = retention_chunkwise(q_r, k_r, v_r, log_gamma)
        ret = rms_norm(ret, ret_norm_g[l]).reshape(B, S, RET_V)
        ret = (jax.nn.silu(rg.astype(jnp.float32)) * ret).astype(x.dtype)
        swa = sliding_window_gqa_sinks(sq.reshape(B, S, SWA_Q_HEADS, SWA_HEAD_DIM),
                                       sk.reshape(B, S, SWA_KV_HEADS, SWA_HEAD_DIM),
                                       sv.reshape(B, S, SWA_KV_HEADS, SWA_HEAD_DIM),
                                       swa_sinks[l])
        merged = jax.nn.sigmoid(a_ret) * (ret @ w_ret_o[l]) + jax.nn.sigmoid(a_swa) * (swa @ w_swa_o[l])
        x = x + merged @ w_mix_o[l]
        x = x + memory_cross_attention(rms_norm(x, xattn_norm_g[l]), rms_norm(mem, mem_norm_g[l]),
                                       w_xq[l], w_xkv[l], w_xo[l])
        x = x + hierarchical_moe(rms_norm(x, moe_norm_g[l]), w_router_group[l], b_router_group[l],
                                 w_router_expert[l], b_router_expert[l],
                                 w_exp_gate[l], w_exp_up[l], w_exp_down[l])
    return rms_norm(x, final_norm_g)
```

```python
import math
from contextlib import ExitStack
import numpy as np
import ml_dtypes
import concourse.bass as bass
import concourse.mybir as mybir
from concourse.bass_utils import run_bass_kernel_spmd

F32 = mybir.dt.float32
BF16 = mybir.dt.bfloat16
AF = mybir.ActivationFunctionType
ALU = mybir.AluOpType
AX = mybir.AxisListType
ENGS = ["tensor", "vector", "scalar", "gpsimd", "sync"]
EPOCH = 30000
RMS_EPS = 1e-6
ROPE_BASE = 10000.0

FULL = dict(D=4096, B=2, SEQ=8192, H=16, HQ=64, HKV=8, XH=4, ML=256, NG=8, EPG=8, DE=512, NC=4)


class Cfg:
    def __init__(self, **kw):
        self.__dict__.update(kw)
        c = self
        c.NC = getattr(c, 'NC', 8)
        c.NS = c.NC // c.B
        c.NW = c.NC if c.NC == 8 else 1
        c.NSG = getattr(c, 'NSG', {8: 1, 4: 2, 2: 4}[c.NC])
        c.T = c.SEQ // (c.NS * c.NSG)
        c.NHS = (c.NS - 1) * c.NSG if c.NW == 1 else 0
        c.NCH = c.T // 128
        c.KD = c.D // 128
        c.QK = c.H * 128
        c.RV = c.H * 256
        c.SQ = c.HQ * 64
        c.SKV = c.HKV * 64
        c.G = c.HQ // c.HKV
        c.XW = c.XH * 256
        c.NE = c.NG * c.EPG
        c.NR = c.NG + c.NE
        c.EL = c.NE // c.NW
        c.widths = [c.QK, c.QK, c.RV, c.RV, c.SQ, c.SKV, c.SKV, c.D, c.D]
        c.offs = [0] + [int(v) for v in np.cumsum(c.widths)]
        c.INW = c.offs[-1]
        c.CW = 5120 if c.INW % 5120 == 0 else c.INW
        c.TR = min(c.T, getattr(c, 'TRmax', 1024))
        c.TT = min(c.T, getattr(c, 'TTmax', 512))
        c.TQ = min(c.T, getattr(c, 'TQmax', 512))


class KB:
    def __init__(self, nc, stack):
        self.nc = nc
        self.stack = stack
        self.count = {e: 0 for e in ENGS}
        self.waited = {e: {} for e in ENGS}
        self.res = {}
        self.sems = {}
        self.semval = {}
        self.nops = 0

    def sem(self, key):
        if key not in self.sems:
            self.sems[key] = self.stack.enter_context(self.nc.semaphore("s%d" % len(self.sems)))
        return self.sems[key]

    def op(self, eng, fn, reads=(), writes=(), dma=None, dma_inc=16):
        need = {}
        for r in reads:
            st = self.res.get(r)
            if st:
                for s, v in st[0].items():
                    if need.get(s, 0) < v:
                        need[s] = v
        for w in writes:
            st = self.res.get(w)
            if st:
                for d in st:
                    for s, v in d.items():
                        if need.get(s, 0) < v:
                            need[s] = v
        h = getattr(self.nc, eng)
        wt = self.waited[eng]
        for s, v in need.items():
            if wt.get(s, 0) < v:
                wt[s] = v
                h.wait_ge(self.sems[s], v)
        if dma is None:
            c = self.count[eng]
            self.count[eng] = c + 1
            ev = ("%s%d" % (eng, c // EPOCH), c % EPOCH + 1)
            inc = 1
        else:
            k = "dma_" + dma
            ev = (k, self.semval.get(k, 0) + dma_inc)
            inc = dma_inc
        self.semval[ev[0]] = ev[1]
        ins = fn(h)
        ins.then_inc(self.sem(ev[0]), inc)
        self.nops += 1
        for r in reads:
            st = self.res.setdefault(r, ({}, {}))
            if st[1].get(ev[0], 0) < ev[1]:
                st[1][ev[0]] = ev[1]
        for w in writes:
            self.res[w] = ({ev[0]: ev[1]}, {})
        return ev

    def barrier(self):
        for e in ENGS:
            h = getattr(self.nc, e)
            wt = self.waited[e]
            for s, v in self.semval.items():
                if wt.get(s, 0) < v:
                    wt[s] = v
                    h.wait_ge(self.sems[s], v)
        self.res = {}


def build(c, dbg=()):
    import os
    STOP = int(os.environ.get('KSTOP', '99'))
    NOAG = bool(os.environ.get('KNOAG'))
    nc = bass.Bass("TRN2", target_bir_lowering=False)
    D, T, NCH, KD, H = c.D, c.T, c.NCH, c.KD, c.H
    QK, RV, SQ, SKV, G, HQ, HKV = c.QK, c.RV, c.SQ, c.SKV, c.G, c.HQ, c.HKV
    XW, XH, ML, NE, NR, NG, EPG, DE, EL = c.XW, c.XH, c.ML, c.NE, c.NR, c.NG, c.EPG, c.DE, c.EL
    NW, NS, NCORES = c.NW, c.NS, c.NC
    TR, TT, TQ = c.TR, c.TT, c.TQ
    XK = XW // 128
    HT = DE // 128

    def din(name, shape, dt=F32):
        return nc.dram_tensor(name, list(shape), dt, kind="ExternalInput").ap()

    def dscr(name, shape, dt):
        return nc.dram_tensor(name, list(shape), dt, kind="Internal").ap()

    NSG = c.NSG
    x_all = din("x", [NSG * T, D])
    xh_all = din("xh", [NSG, 128, D])
    NHS = c.NHS
    xhist = din("xhist", [max(NHS, 1) * T, D])
    cs_hist = din("cs_h", [max(NHS, 1), 128, NCH, 4, 64])
    mem_in = din("mem", [ML, D])
    g_mix = din("g_mix", [1, D]); g_xat = din("g_xat", [1, D]); g_mem = din("g_mem", [1, D])
    g_moe = din("g_moe", [1, D]); g_fin = din("g_fin", [1, D]); g_ret = din("g_ret", [1, RV])
    sinks_in = din("sinks", [1, HQ]); b_r = din("b_r", [1, NR])
    w_in_sh = din("w_in", [D // NW, c.INW]); w_ro_sh = din("w_ro", [RV // NW, D]); w_so_sh = din("w_so", [SQ // NW, D])
    w_mo_sh = din("w_mo", [D // NW, D]); w_xq_sh = din("w_xq", [D // NW, XW]); w_xkv_sh = din("w_xkv", [D // NW, 2 * XW])
    w_xo_sh = din("w_xo", [XW // NW, D]); w_r_sh = din("w_r", [D // NW, NR])
    w_g_sh = din("w_g", [EL * D, DE]); w_u_sh = din("w_u", [EL * D, DE]); w_d_sh = din("w_d", [EL * DE, D])
    cs_all = din("cs", [NSG, 128, NCH, 4, 64]); dec_in = din("dec", [128, H, 128]); qkd_in = din("qkd", [128, 3, H])
    coef_in = din("coef", [128, 8, H]); msk_in = din("msk", [128, 3, 512], BF16)
    idb_in = din("idb", [128, 128], BF16); idf_in = din("idf", [128, 128]); ones_in = din("ones", [128, 128], BF16)
    y_all = nc.dram_tensor("y", [NSG * T, D], F32, kind="ExternalOutput").ap()
    RST = dscr("RST", [128, H * 256], F32)

    RQ = dscr("RQ", [T, QK], BF16); RK = dscr("RK", [T, QK], BF16); RVs = dscr("RVs", [T, RV], BF16)
    RG = dscr("RG", [T, RV], F32)
    SQT = dscr("SQT", [HQ, 64, T], BF16); SKT = dscr("SKT", [HKV, 64, 128 + T], BF16)
    SV = dscr("SV", [128 + T, SKV], BF16)
    AR = dscr("AR", [D, T], F32); AS = dscr("AS", [D, T], F32)
    RETT = dscr("RETT", [RV, T], BF16); SWAT = dscr("SWAT", [SQ, T], BF16)
    M1 = dscr("M1", [D, T], F32); MT = dscr("MT", [D, T], BF16)
    X2 = dscr("X2", [T, D], F32); X3 = dscr("X3", [T, D], F32)
    SLOC = dscr("SLOC", [128, H * 256], F32); SGA = dscr("SGA", [8 * 128, H * 256], F32)
    WTD = dscr("WTD", [T // TQ, NE, TQ], F32)
    XNT = dscr("XNT", [D, T], BF16)
    YT = dscr("YT", [D, T], F32)

    dbg_outs = {}

    with ExitStack() as st:
        kb = KB(nc, st)
        st.enter_context(nc.Block())
        for e in ENGS:
            kb.sem("%s0" % e)
        op = kb.op
        uid = [0]

        def sb(stack, name, shape, dt=F32):
            uid[0] += 1
            return stack.enter_context(nc.sbuf_tensor("%s_%d" % (name, uid[0]), list(shape), dt))

        NPSF = int(os.environ.get('KPS', '6'))
        psf = [st.enter_context(nc.psum_tensor("psf%d" % i, [128, 512], F32)) for i in range(NPSF)]
        psb = [st.enter_context(nc.psum_tensor("psb%d" % i, [128, 512], BF16)) for i in range(2)]
        rr = {"f": 0, "b": 0}

        def nps():
            i = rr["f"]; rr["f"] = (i + 1) % NPSF
            return psf[i], "psf%d" % i

        def npb():
            i = rr["b"]; rr["b"] = (i + 1) % 2
            return psb[i], "psb%d" % i

        idb = sb(st, "idb", [128, 128], BF16); idf = sb(st, "idf", [128, 128]); ones = sb(st, "ones", [128, 128], BF16)
        op("sync", lambda e: e.dma_start(out=idb[:], in_=idb_in[:, :]), writes=["idb"], dma="const")
        op("sync", lambda e: e.nop(), reads=["idb"])
        op("sync", lambda e: e.dma_start(out=idf[:], in_=idf_in[:, :]), writes=["idf"], dma="const")
        op("sync", lambda e: e.nop(), reads=["idf"])
        op("sync", lambda e: e.dma_start(out=ones[:], in_=ones_in[:, :]), writes=["ones"], dma="const")
        op("sync", lambda e: e.nop(), reads=["ones"])

        lastcc = []
        SERCC = not os.environ.get('KPARCC')

        def prep(name, src, rows_sh, col_chunks=None, row_chunks=None):
            outs = []
            if NOAG:
                if col_chunks is None:
                    col_chunks = [(0, src.shape[1])]
                if row_chunks is None:
                    row_chunks = [(0, rows_sh)]
                ci = 0
                for (r0, r1) in row_chunks:
                    for (c0, c1) in col_chunks:
                        wgd = din("dbgw_%s_%d" % (name, ci), [8 * (r1 - r0), c1 - c0], BF16)
                        outs.append((wgd, ("W", name, ci)))
                        ci += 1
                return outs
            if col_chunks is None:
                col_chunks = [(0, src.shape[1])]
            if row_chunks is None:
                row_chunks = [(0, rows_sh)]
            ci = 0
            for (r0, r1) in row_chunks:
                for (c0, c1) in col_chunks:
                    nr, ncol = r1 - r0, c1 - c0
                    wb = dscr("wb_%s_%d" % (name, ci), [nr, ncol], BF16)
                    wg = dscr("wg_%s_%d" % (name, ci), [8 * nr, ncol], BF16) if NW > 1 else wb
                    rp = max(1, min(nr, (4 << 20) // (ncol * 4)))
                    keys = []
                    for p0 in range(0, nr, rp):
                        p1 = min(nr, p0 + rp)
                        key = ("wb", name, ci, p0)
                        keys.append(key)
                        op("gpsimd", lambda e, p0=p0, p1=p1, wb=wb: e.dma_start(out=wb[p0:p1, :], in_=src[r0 + p0:r0 + p1, c0:c1]),
                           writes=[key], dma="cast")
                    if NW > 1:
                        op("gpsimd", lambda e, wb=wb, wg=wg: e.collective_compute(
                            "AllGather", ALU.bypass, replica_groups=[list(range(NCORES))], ins=[wb.opt()], outs=[wg.opt()]),
                           reads=keys + lastcc, writes=[("W", name, ci)], dma="cc", dma_inc=1)
                        lastcc[:] = [("W", name, ci)] if SERCC else []
                    else:
                        op("gpsimd", lambda e: e.nop(), reads=keys, writes=[("W", name, ci)])
                    outs.append((wg, ("W", name, ci)))
                    ci += 1
            return outs

        Win = prep("in", w_in_sh, D // NW, col_chunks=[(i, i + c.CW) for i in range(0, c.INW, c.CW)])
        Wro = prep("ro", w_ro_sh, RV // NW)[0]
        Wso = prep("so", w_so_sh, SQ // NW)[0]
        Wmo = prep("mo", w_mo_sh, D // NW)[0]
        Wxkv = prep("xkv", w_xkv_sh, D // NW)[0]
        Wxq = prep("xq", w_xq_sh, D // NW)[0]
        Wxo = prep("xo", w_xo_sh, XW // NW)[0]
        Wr = prep("r", w_r_sh, D // NW)[0]
        Wg = prep("g", w_g_sh, EL * D, row_chunks=[(j * D, (j + 1) * D) for j in range(EL)])
        Wu = prep("u", w_u_sh, EL * D, row_chunks=[(j * D, (j + 1) * D) for j in range(EL)])
        Wd = prep("d", w_d_sh, EL * DE, row_chunks=[(j * DE, (j + 1) * DE) for j in range(EL)])

        class NormT:
            def __init__(self, ph, tag):
                self.tag = tag
                self.xin = [sb(ph, "xin", [128, D]) for _ in range(2)]
                self.junk = sb(ph, "junk", [128, D], BF16)
                self.hb = sb(ph, "hb", [128, D], BF16)
                self.ss = sb(ph, "ss", [128, 1]); self.ms = sb(ph, "ms", [128, 1])
                self.sd = sb(ph, "sd", [128, 1]); self.rs = sb(ph, "rs", [128, 1])
                self.n = 0

            def run(self, src, nchunks, gain, gain_res, dstT, dst_res, tok_off=0):
                tag = self.tag
                junk, hb, ss, ms, sd, rs = self.junk, self.hb, self.ss, self.ms, self.sd, self.rs
                keys = []
                for ci in range(nchunks):
                    b = self.n % 2
                    self.n += 1
                    xin = self.xin[b]
                    xr = "%sxin%d" % (tag, b)
                    op("sync", lambda e: e.dma_start(out=xin[:], in_=src[ci * 128:(ci + 1) * 128, :]), writes=[xr], dma=xr)
                    KN = int(os.environ.get('KN', '9'))
                    if KN < 1:
                        continue
                    op("scalar", lambda e: e.activation(out=junk[:], in_=xin[:], func=AF.Square, accum_out=ss[:]),
                       reads=[xr], writes=[tag + "junk", tag + "ss"])
                    op("vector", lambda e: e.tensor_scalar(out=ms[:], in0=ss[:], scalar1=1.0 / D, scalar2=RMS_EPS, op0=ALU.mult, op1=ALU.add),
                       reads=[tag + "ss"], writes=[tag + "ms"])
                    op("scalar", lambda e: e.activation(out=sd[:], in_=ms[:], func=AF.Sqrt), reads=[tag + "ms"], writes=[tag + "sd"])
                    op("vector", lambda e: e.reciprocal(out=rs[:], in_=sd[:]), reads=[tag + "sd"], writes=[tag + "rs"])
                    if KN < 2:
                        continue
                    op("vector", lambda e: e.scalar_tensor_tensor(out=hb[:], in0=xin[:], scalar=rs[:, 0:1], in1=gain[:], op0=ALU.mult, op1=ALU.mult),
                       reads=[xr, tag + "rs", gain_res], writes=[tag + "hb"])
                    if KN < 3:
                        continue
                    for k4 in range(0, KD, 4):
                        nk = min(4, KD - k4)
                        pt, pr = npb()

                        def tr(e):
                            for j in range(nk):
                                i = e.transpose(pt[:, j * 128:(j + 1) * 128], hb[:, (k4 + j) * 128:(k4 + j + 1) * 128], idb[:])
                            return i
                        op("tensor", tr, reads=[tag + "hb", "idb"], writes=[pr])
                        t0 = tok_off + ci * 128
                        dst = dstT[:, k4:k4 + nk, t0:t0 + 128]
                        srcp = pt[:, 0:nk * 128].rearrange("p (c t) -> p c t", c=nk)
                        key = (dst_res, t0, k4)
                        for j in range(nk):
                            if os.environ.get('KALLDVE'):
                                op("vector", lambda e: e.tensor_copy(out=dstT[:, k4 + j, t0:t0 + 128], in_=pt[:, j * 128:(j + 1) * 128]), reads=[pr], writes=[key + (j,)])
                            else:
                                op("scalar", lambda e: e.activation(out=dstT[:, k4 + j, t0:t0 + 128], in_=pt[:, j * 128:(j + 1) * 128], func=AF.Copy), reads=[pr], writes=[key + (j,)])
                            keys.append(key + (j,))
                return keys

        def load_gain(ph, src, n, name):
            g = sb(ph, name, [128, n])
            op("sync", lambda e: e.dma_start(out=g[:], in_=src[0:1, :].partition_broadcast(128)), writes=[name], dma="const")
            op("sync", lambda e: e.nop(), reads=[name])
            return g

        class WStream:
            def __init__(self, ph, KCmax, WC, nbuf, tag):
                self.t = [sb(ph, "wt" + tag, [128, KCmax, WC], BF16) for _ in range(nbuf)]
                self.i = 0
                self.tag = tag

            def load(self, W, wres, KC, c0, ncols, r0=0):
                b = self.i % len(self.t)
                self.i += 1
                t = self.t[b]
                res = "wt%s%d" % (self.tag, b)
                src = W[r0:r0 + KC * 128, c0:c0 + ncols].rearrange("(c p) n -> p c n", p=128)
                op("sync", lambda e: e.dma_start(out=t[:, 0:KC, 0:ncols], in_=src), reads=[wres], writes=[res], dma=res)
                return t, res

        def mm_group(ps, pres, pairs, reads):
            n = len(pairs)

            def f(e):
                for i, (l, r) in enumerate(pairs):
                    ins = e.matmul(ps, l, r, start=(i == 0), stop=(i == n - 1))
                return ins
            op("tensor", f, reads=reads, writes=[pres])

        for gseg in range(NHS + c.NSG):
            hist = gseg < NHS
            seg = 0 if hist else gseg - NHS
            x_in = xhist[gseg * T:(gseg + 1) * T, :] if hist else x_all[seg * T:(seg + 1) * T, :]
            xh_in = xh_all[seg]
            cs_in = cs_hist[gseg] if hist else cs_all[seg]
            y_out = y_all[seg * T:(seg + 1) * T, :]
            if STOP >= 2:
                o_rq, o_rk, o_rv, o_rg, o_sq, o_sk, o_sv, o_ar, o_as = c.offs[:9]
                with ExitStack() as ph:
                    gmix = load_gain(ph, g_mix, D, "gmix")
                    cs = sb(ph, "cs", [128, NCH, 4, 64])
                    if not os.environ.get('KNOCS'):
                        op("sync", lambda e: e.dma_start(out=cs[:], in_=cs_in[:, :, :, :]), writes=["cs"], dma="const")
                        op("sync", lambda e: e.nop(), reads=["cs"])
                    hT = sb(ph, "hT", [128, KD, TR], BF16)
                    hTh = sb(ph, "hTh", [128, KD, 128], BF16)
                    ws = WStream(ph, KD, 256, 2, "a")
                    nt = NormT(ph, "n1")
                    stg = [sb(ph, "stg", [128, 512]) for _ in range(3)]
                    stb = [sb(ph, "stb", [128, 512], BF16) for _ in range(3)]
                    rt = [sb(ph, "rt", [128, 4, 128]) for _ in range(2)]
                    sctr = {"g": 0, "b": 0, "r": 0}

                    def nstg():
                        i = sctr["g"]; sctr["g"] = (i + 1) % 3
                        return stg[i], "stg%d" % i

                    def nstb():
                        i = sctr["b"]; sctr["b"] = (i + 1) % 3
                        return stb[i], "stb%d" % i

                    KP1 = os.environ.get('KP1', 'z')
                    halo_keys = nt.run(xh_in, 1, gmix, "gmix", hTh, "hTh") if (KP1 >= 'b' and not hist) else []
                    for half in range(T // TR if KP1 >= 'c' else 0):
                        tok0 = half * TR
                        hkeys = nt.run(x_in[tok0:tok0 + TR, :], TR // 128, gmix, "gmix", hT, "hT")
                        for c0 in range(0, c.INW, 256):
                            slot = max(i for i in range(9) if c.offs[i] <= c0)
                            if os.environ.get('KSLOTS') is not None and str(slot) not in os.environ['KSLOTS']:
                                continue
                            if hist and slot not in (1, 2):
                                continue
                            lc = c0 - c.offs[slot]
                            Wc, wres = Win[c0 // c.CW]
                            wt, wtr = ws.load(Wc, wres, KD, c0 % c.CW, 256)
                            if slot in (0, 1, 2, 3, 6):
                                chunks = list(range(TR // 128))
                                if slot == 6 and half == 0:
                                    chunks = [-1] + chunks
                                for tc in chunks:
                                    ps, pr = nps()
                                    if tc < 0:
                                        pairs = [(hTh[:, k, :], wt[:, k, 0:256]) for k in range(KD)]
                                        rd = halo_keys + [wtr]
                                    else:
                                        pairs = [(hT[:, k, tc * 128:(tc + 1) * 128], wt[:, k, 0:256]) for k in range(KD)]
                                        rd = hkeys + [wtr]
                                    mm_group(ps[:, 0:256], pr, pairs, rd)
                                    n_glob = (tok0 // 128) + tc
                                    rows = slice(tok0 + tc * 128, tok0 + (tc + 1) * 128)
                                    if slot in (0, 1):
                                        ob, obr = nstb()
                                        ri = sctr["r"]; sctr["r"] = (ri + 1) % 2
                                        r_t, rres = rt[ri], "rt%d" % ri
                                        pv = ps[:, 0:256].rearrange("p (h t d) -> p h t d", h=2, t=2)
                                        x1, x2 = pv[:, :, 0, :], pv[:, :, 1, :]
                                        co = cs[:, n_glob, 2 * slot, :].unsqueeze(1).to_broadcast([128, 2, 64])
                                        si = cs[:, n_glob, 2 * slot + 1, :].unsqueeze(1).to_broadcast([128, 2, 64])
                                        rv4 = r_t[:].rearrange("p a (h d) -> p a h d", h=2)
                                        for j, (a, bb) in enumerate([(x1, co), (x2, si), (x1, si), (x2, co)]):
                                            op("vector", lambda e, j=j, a=a, bb=bb: e.tensor_tensor(out=rv4[:, j], in0=a, in1=bb, op=ALU.mult),
                                               reads=[pr, "cs"], writes=[(rres, j)])
                                        ov = ob[:, 0:256].rearrange("p (h t d) -> p h t d", h=2, t=2)
                                        op("gpsimd", lambda e: e.tensor_tensor(out=ov[:, :, 0, :], in0=rv4[:, 0], in1=rv4[:, 1], op=ALU.subtract),
                                           reads=[(rres, 0), (rres, 1)], writes=[(obr, 0)])
                                        op("gpsimd", lambda e: e.tensor_tensor(out=ov[:, :, 1, :], in0=rv4[:, 2], in1=rv4[:, 3], op=ALU.add),
                                           reads=[(rres, 2), (rres, 3)], writes=[(obr, 1)])
                                        dst = (RQ if slot == 0 else RK)[rows, lc:lc + 256]
                                        op("scalar", lambda e: e.dma_start(out=dst, in_=ob[:, 0:256]), reads=[(obr, 0), (obr, 1)],
                                           writes=[("RQ" if slot == 0 else "RK", n_glob, lc), obr], dma=obr)
                                    elif slot == 2:
                                        ob, obr = nstb()
                                        op("scalar", lambda e: e.activation(out=ob[:, 0:256], in_=ps[:, 0:256], func=AF.Copy), reads=[pr], writes=[obr])
                                        op("scalar", lambda e: e.dma_start(out=RVs[rows, lc:lc + 256], in_=ob[:, 0:256]), reads=[obr],
                                           writes=[("RV", n_glob, lc)], dma=obr)
                                    elif slot == 3:
                                        og, ogr = nstg()
                                        op("scalar", lambda e: e.activation(out=og[:, 0:256], in_=ps[:, 0:256], func=AF.Silu), reads=[pr], writes=[ogr])
                                        op("scalar", lambda e: e.dma_start(out=RG[rows, lc:lc + 256], in_=og[:, 0:256]), reads=[ogr],
                                           writes=[("RG", n_glob, lc)], dma=ogr)
                                    else:
                                        ob, obr = nstb()
                                        op("vector", lambda e: e.tensor_copy(out=ob[:, 0:256], in_=ps[:, 0:256]), reads=[pr], writes=[obr])
                                        r2 = slice(128 + tok0 + tc * 128, 128 + tok0 + (tc + 1) * 128) if tc >= 0 else slice(0, 128)
                                        op("scalar", lambda e: e.dma_start(out=SV[r2, lc:lc + 256], in_=ob[:, 0:256]), reads=[obr],
                                           writes=[("SV", n_glob, lc)], dma=obr)
                            else:
                                M = 64 if slot in (4, 5) else 128
                                for m in range(256 // M):
                                    tiles = [(tt, TT) for tt in range(TR // TT)]
                                    if slot == 5 and half == 0:
                                        tiles = [(-1, 128)] + tiles
                                    for (tt, ntk) in tiles:
                                        ps, pr = nps()
                                        if tt < 0:
                                            pairs = [(wt[:, k, m * M:(m + 1) * M], hTh[:, k, :]) for k in range(KD)]
                                            rd = halo_keys + [wtr]
                                        else:
                                            pairs = [(wt[:, k, m * M:(m + 1) * M], hT[:, k, tt * TT:(tt + 1) * TT]) for k in range(KD)]
                                            rd = hkeys + [wtr]
                                        mm_group(ps[0:M, 0:ntk], pr, pairs, rd)
                                        fi = (lc + m * M) // M
                                        tsl = slice(tok0 + tt * TT, tok0 + (tt + 1) * TT)
                                        if slot == 4:
                                            ob, obr = nstb()
                                            op("scalar", lambda e: e.activation(out=ob[0:64, 0:ntk], in_=ps[0:64, 0:ntk], func=AF.Copy),
                                               reads=[pr], writes=[obr])
                                            op("scalar", lambda e: e.dma_start(out=SQT[fi, :, tsl], in_=ob[0:64, 0:ntk]), reads=[obr],
                                               writes=[("SQT", fi, half, tt)], dma=obr)
                                        elif slot == 5:
                                            ob, obr = nstb()
                                            op("vector", lambda e: e.tensor_copy(out=ob[0:64, 0:ntk], in_=ps[0:64, 0:ntk]), reads=[pr], writes=[obr])
                                            ks = slice(128 + tok0 + tt * TT, 128 + tok0 + (tt + 1) * TT) if tt >= 0 else slice(0, 128)
                                            op("scalar", lambda e: e.dma_start(out=SKT[fi, :, ks], in_=ob[0:64, 0:ntk]), reads=[obr],
                                               writes=[("SKT", fi, half, tt)], dma=obr)
                                        else:
                                            og, ogr = nstg()
                                            op("scalar", lambda e: e.activation(out=og[:, 0:ntk], in_=ps[:, 0:ntk], func=AF.Sigmoid), reads=[pr], writes=[ogr])
                                            dstt = (AR if slot == 7 else AS)[fi * 128:(fi + 1) * 128, tsl]
                                            op("scalar", lambda e: e.dma_start(out=dstt, in_=og[:, 0:ntk]), reads=[ogr],
                                               writes=[("AR" if slot == 7 else "AS", fi, half, tt)], dma=ogr)
                kb.barrier()

            if STOP >= 3:
                with ExitStack() as ph:
                    dec = sb(ph, "dec", [128, H, 128])
                    qkd = sb(ph, "qkd", [128, 3, H])
                    coef = sb(ph, "coef", [128, 8, H])
                    gret = load_gain(ph, g_ret, RV, "gret")
                    op("sync", lambda e: e.dma_start(out=dec[:], in_=dec_in[:, :, :]), writes=["dec"], dma="const")
                    op("sync", lambda e: e.nop(), reads=["dec"])
                    op("sync", lambda e: e.dma_start(out=qkd[:], in_=qkd_in[:, :, :]), writes=["qkd"], dma="const")
                    op("sync", lambda e: e.nop(), reads=["qkd"])
                    op("sync", lambda e: e.dma_start(out=coef[:], in_=coef_in[:, :, :]), writes=["coef"], dma="const")
                    op("sync", lambda e: e.nop(), reads=["coef"])
                    R = [sb(ph, "R", [128, H, 256]) for _ in range(2)]
                    Rb = sb(ph, "Rb", [128, H, 256], BF16)
                    qt = [sb(ph, "qt", [128, QK], BF16) for _ in range(2)]
                    kt = [sb(ph, "kt", [128, QK], BF16) for _ in range(2)]
                    vt = [sb(ph, "vt", [128, RV], BF16) for _ in range(2)]
                    gt = [sb(ph, "gt", [128, RV]) for _ in range(2)]
                    kdc = [sb(ph, "kdc", [128, H, 128], BF16) for _ in range(2)]
                    qkT = [sb(ph, "qkT", [128, 256], BF16) for _ in range(2)]
                    PT = [sb(ph, "PT", [128, 128], BF16) for _ in range(2)]
                    Bs = [sb(ph, "Bs", [128, 256]) for _ in range(2)]
                    oc = sb(ph, "oc", [128, H, 256])
                    yb = sb(ph, "yb", [128, RV], BF16)
                    ssr = sb(ph, "ssr", [128, H]); msr = sb(ph, "msr", [128, H]); sdr = sb(ph, "sdr", [128, H]); rsr = sb(ph, "rsr", [128, H])
                    jr = sb(ph, "jr", [128, 256], BF16)
                    retS = [sb(ph, "retS", [128, RV // 128, 256], BF16) for _ in range(2)]
                    sg_t = gt

                    def load_kv(n, need_q):
                        b = n % 2
                        rows = slice(n * 128, (n + 1) * 128)
                        rd_k = [("RK", n, l) for l in range(0, QK, 256)]
                        rd_v = [("RV", n, l) for l in range(0, RV, 256)]
                        op("sync", lambda e: e.dma_start(out=kt[b][:], in_=RK[rows, :]), reads=rd_k, writes=["kt%d" % b], dma="kt%d" % b)
                        op("sync", lambda e: e.dma_start(out=vt[b][:], in_=RVs[rows, :]), reads=rd_v, writes=["vt%d" % b], dma="vt%d" % b)
                        if need_q:
                            rd_q = [("RQ", n, l) for l in range(0, QK, 256)]
                            rd_g = [("RG", n, l) for l in range(0, RV, 256)]
                            op("sync", lambda e: e.dma_start(out=qt[b][:], in_=RQ[rows, :]), reads=rd_q, writes=["qt%d" % b], dma="qt%d" % b)
                            op("sync", lambda e: e.dma_start(out=gt[b][:], in_=RG[rows, :]), reads=rd_g, writes=["gt%d" % b], dma="gt%d" % b)
                        op("gpsimd", lambda e: e.tensor_tensor(out=kdc[b][:], in0=kt[b][:].rearrange("p (h d) -> p h d", h=H),
                                                               in1=qkd[:, 1, :].unsqueeze(2).to_broadcast([128, H, 128]), op=ALU.mult),
                           reads=["kt%d" % b, "qkd"], writes=["kdc%d" % b])
                        return b

                    def state_update(b, cur, h):
                        ps, pr = nps()
                        mm_group(ps[:, 0:256], pr, [(kdc[b][:, h, :], vt[b][:, h * 256:(h + 1) * 256])], ["kdc%d" % b, "vt%d" % b])
                        op("vector", lambda e: e.scalar_tensor_tensor(out=R[1 - cur][:, h, :], in0=R[cur][:, h, :], scalar=qkd[:, 2, h:h + 1],
                                                                      in1=ps[:, 0:256], op0=ALU.mult, op1=ALU.add),
                           reads=[pr, ("R", cur, h), "qkd"], writes=[("R", 1 - cur, h)])

                    if NW == 1 and gseg > 0:
                        op("sync", lambda e: e.dma_start(out=R[0][:].rearrange("p h d -> p (h d)"), in_=RST[:, :]), writes=[("R", 0, h) for h in range(H)], dma="rst")
                    else:
                        for h in range(H):
                            op("gpsimd", lambda e, h=h: e.memset(R[0][:, h, :], 0.0), writes=[("R", 0, h)])
                    cur = 0
                    for n in range(NCH if ((NW > 1 and NS > 1) or hist) else 0):
                        b = load_kv(n, False)
                        for h in range(H):
                            state_update(b, cur, h)
                        cur = 1 - cur
                    if NW > 1 and NS > 1:
                        op("sync", lambda e: e.dma_start(out=SLOC[:, :], in_=R[cur][:].rearrange("p h d -> p (h d)")),
                           reads=[("R", cur, h) for h in range(H)], writes=["SLOC"], dma="sloc")
                        if not NOAG:
                            op("gpsimd", lambda e: e.collective_compute("AllGather", ALU.bypass, replica_groups=[list(range(NCORES))],
                                                                        ins=[SLOC.opt()], outs=[SGA.opt()]),
                               reads=["SLOC"], writes=["SGA"], dma="cc", dma_inc=1)
                        else:
                            for j in range(8):
                                op("sync", lambda e: e.dma_start(out=SGA[j * 128:(j + 1) * 128, :], in_=SLOC[:, :]), reads=["SLOC"], writes=[("SGA", j)], dma="sgad")
                        cur2 = 1 - cur
                        for h in range(H):
                            op("gpsimd", lambda e, h=h: e.memset(R[cur2][:, h, :], 0.0), writes=[("R", cur2, h)])
                        for j in range(8):
                            sgb = sg_t[j % 2]
                            sgr = "gt%d" % (j % 2)
                            op("sync", lambda e: e.dma_start(out=sgb[:], in_=SGA[j * 128:(j + 1) * 128, :]), reads=["SGA", ("SGA", j)], writes=[sgr], dma=sgr)
                            for h in range(H):
                                op("vector", lambda e, h=h: e.scalar_tensor_tensor(out=R[1 - cur2][:, h, :], in0=sgb[:, h * 256:(h + 1) * 256],
                                                                                   scalar=coef[:, j, h:h + 1], in1=R[cur2][:, h, :],
                                                                                   op0=ALU.mult, op1=ALU.add),
                                   reads=[sgr, "coef", ("R", cur2, h)], writes=[("R", 1 - cur2, h)])
                            cur2 = 1 - cur2
                        cur = cur2
                    for h in range(H if not hist else 0):
                        op("scalar", lambda e, h=h: e.activation(out=Rb[:, h, :], in_=R[cur][:, h, :], func=AF.Copy),
                           reads=[("R", cur, h)], writes=[("Rb", h)])
                    for n in range(NCH if not hist else 0):
                        b = load_kv(n, True)
                        for h in range(H):
                            i2 = h % 2
                            pt, ptr = npb()

                            def tr(e, pt=pt, h=h):
                                e.transpose(pt[:, 0:128], qt[b][:, h * 128:(h + 1) * 128], idb[:])
                                return e.transpose(pt[:, 128:256], kt[b][:, h * 128:(h + 1) * 128], idb[:])
                            op("tensor", tr, reads=["qt%d" % b, "kt%d" % b, "idb"], writes=[ptr])
                            op("scalar", lambda e, pt=pt: e.activation(out=qkT[i2][:], in_=pt[:, 0:256], func=AF.Copy), reads=[ptr], writes=["qkT%d" % i2])
                            psA, prA = nps()
                            mm_group(psA[:, 0:128], prA, [(qkT[i2][:, 128:256], qkT[i2][:, 0:128])], ["qkT%d" % i2])
                            op("vector", lambda e, psA=psA, h=h: e.tensor_tensor(out=PT[i2][:], in0=psA[:, 0:128], in1=dec[:, h, :], op=ALU.mult),
                               reads=[prA, "dec"], writes=["PT%d" % i2])
                            psB, prB = nps()
                            mm_group(psB[:, 0:256], prB, [(PT[i2][:], vt[b][:, h * 256:(h + 1) * 256])], ["PT%d" % i2, "vt%d" % b])
                            psC, prC = nps()
                            mm_group(psC[:, 0:256], prC, [(qkT[i2][:, 0:128], Rb[:, h, :])], ["qkT%d" % i2, ("Rb", h)])
                            op("scalar", lambda e, psB=psB: e.activation(out=Bs[i2][:], in_=psB[:, 0:256], func=AF.Copy), reads=[prB], writes=["Bs%d" % i2])
                            op("vector", lambda e, psC=psC, h=h: e.scalar_tensor_tensor(out=oc[:, h, :], in0=psC[:, 0:256], scalar=qkd[:, 0, h:h + 1],
                                                                                        in1=Bs[i2][:], op0=ALU.mult, op1=ALU.add),
                               reads=[prC, "Bs%d" % i2, "qkd"], writes=[("oc", h)])
                            state_update(b, cur, h)
                            op("scalar", lambda e, h=h: e.activation(out=Rb[:, h, :], in_=R[1 - cur][:, h, :], func=AF.Copy),
                               reads=[("R", 1 - cur, h)], writes=[("Rb", h)])
                            op("scalar", lambda e, h=h: e.activation(out=jr[:], in_=oc[:, h, :], func=AF.Square, accum_out=ssr[:, h:h + 1]),
                               reads=[("oc", h)], writes=["jr", ("ssr", h)])
                        cur = 1 - cur
                        op("vector", lambda e: e.tensor_scalar(out=msr[:], in0=ssr[:], scalar1=1.0 / 256, scalar2=RMS_EPS, op0=ALU.mult, op1=ALU.add),
                           reads=[("ssr", h) for h in range(H)], writes=["msr"])
                        op("scalar", lambda e: e.activation(out=sdr[:], in_=msr[:], func=AF.Sqrt), reads=["msr"], writes=["sdr"])
                        op("vector", lambda e: e.reciprocal(out=rsr[:], in_=sdr[:]), reads=["sdr"], writes=["rsr"])
                        for h in range(H):
                            op("vector", lambda e, h=h: e.scalar_tensor_tensor(out=oc[:, h, :], in0=oc[:, h, :], scalar=rsr[:, h:h + 1],
                                                                               in1=gret[:, h * 256:(h + 1) * 256], op0=ALU.mult, op1=ALU.mult),
                               reads=[("oc", h), "rsr", "gret"], writes=[("oc", h)])
                        op("gpsimd", lambda e: e.tensor_tensor(out=yb[:], in0=oc[:].rearrange("p h d -> p (h d)"), in1=gt[b][:], op=ALU.mult),
                           reads=[("oc", h) for h in range(H)] + ["gt%d" % b], writes=["yb"])
                        grp = n // 2
                        rS = retS[grp % 2]
                        rSr = "retS%d" % (grp % 2)
                        for k4 in range(0, RV // 128, 4):
                            pt, ptr = npb()

                            def tr2(e, pt=pt, k4=k4):
                                for j in range(4):
                                    i = e.transpose(pt[:, j * 128:(j + 1) * 128], yb[:, (k4 + j) * 128:(k4 + j + 1) * 128], idb[:])
                                return i
                            op("tensor", tr2, reads=["yb", "idb"], writes=[ptr])
                            for j in range(4):
                                dst = rS[:, k4 + j, (n % 2) * 128:(n % 2 + 1) * 128]
                                if False:
                                    pass
                                else:
                                    op("scalar", lambda e: e.activation(out=dst, in_=pt[:, j * 128:(j + 1) * 128], func=AF.Copy), reads=[ptr], writes=[(rSr, n % 2, k4, j)])
                        if n % 2 == 1 or n == NCH - 1:
                            ng = (n % 2) + 1
                            t0 = grp * 256
                            op("scalar", lambda e, rS=rS, t0=t0, ng=ng: e.dma_start(
                                out=RETT[:, t0:t0 + ng * 128].rearrange("(c p) t -> p c t", p=128), in_=rS[:, :, 0:ng * 128]),
                               reads=[(rSr, q, k4, j) for q in range(ng) for k4 in range(0, RV // 128, 4) for j in range(4)],
                               writes=[("RETT", grp)] + [(rSr, q, k4, j) for q in range(ng) for k4 in range(0, RV // 128, 4) for j in range(4)], dma=rSr)
                    if NW == 1 and gseg < NHS + NSG - 1:
                        op("sync", lambda e: e.dma_start(out=RST[:, :], in_=R[cur][:].rearrange("p h d -> p (h d)")),
                           reads=[("R", cur, h) for h in range(H)], writes=["RST"], dma="rst")
                kb.barrier()

            if STOP >= 4 and not hist:
                HB = min(4, G)
                NHB = G // HB
                with ExitStack() as ph:
                    msk = sb(ph, "msk", [128, 3, 512], BF16)
                    op("sync", lambda e: e.dma_start(out=msk[:], in_=msk_in[:, :, :]), writes=["msk"], dma="const")
                    op("sync", lambda e: e.nop(), reads=["msk"])
                    snk = sb(ph, "snk", [128, HQ]); esk = sb(ph, "esk", [128, HQ])
                    op("sync", lambda e: e.dma_start(out=snk[:], in_=sinks_in[0:1, :].partition_broadcast(128)), writes=["snk"], dma="const")
                    op("sync", lambda e: e.nop(), reads=["snk"])
                    op("scalar", lambda e: e.activation(out=esk[:], in_=snk[:], func=AF.Exp), reads=["snk"], writes=["esk"])
                    sq_t = [sb(ph, "sq", [64, G, T], BF16) for _ in range(2)]
                    sk_t = [sb(ph, "sk", [64, 128 + T], BF16) for _ in range(2)]
                    sv_t = [sb(ph, "sv", [128, NCH + 1, 65], BF16) for _ in range(2)]
                    for b in range(2):
                        op("gpsimd", lambda e, b=b: e.memset(sv_t[b][:, :, 64:65], 1.0), writes=["sv%d" % b])
                    Pe = [sb(ph, "Pe", [128, 512], BF16) for _ in range(4)]
                    Pm = [sb(ph, "Pm", [128, 512], BF16) for _ in range(4)]
                    den = [sb(ph, "den", [128, 4]) for _ in range(2)]
                    rec = [sb(ph, "rec", [128, 4]) for _ in range(2)]
                    otk = [sb(ph, "otk", [128, G * 64], BF16) for _ in range(2)]
                    swS = [sb(ph, "swS", [128, max(1, G * 64 // 128), 512], BF16) for _ in range(2)]
                    pctr = [0]
                    dctr = [0]
                    NB = max(1, G * 64 // 128)
                    for g in range(HKV):
                        b = g % 2
                        op("sync", lambda e: e.dma_start(out=sq_t[b][:], in_=SQT[g * G:(g + 1) * G, :, :].rearrange("h d t -> d h t")),
                           reads=[("SQT", g * G + hh, half, tt) for hh in range(G) for half in range(T // TR) for tt in range(TR // TT)],
                           writes=["sq%d" % b], dma="sq%d" % b)
                        op("sync", lambda e: e.dma_start(out=sk_t[b][:], in_=SKT[g, :, :]),
                           reads=[("SKT", g, half, tt) for half in range(T // TR) for tt in range(TR // TT)] + [("SKT", g, 0, -1)],
                           writes=["sk%d" % b], dma="sk%d" % b)
                        lcb = (g * 64) // 256 * 256
                        op("sync", lambda e: e.dma_start(out=sv_t[b][:, :, 0:64], in_=SV[:, g * 64:(g + 1) * 64].rearrange("(c p) d -> p c d", p=128)),
                           reads=[("SV", n, lcb) for n in range(-1, NCH)], writes=["sv%d" % b], dma="sv%d" % b)
                        for n in range(NCH):
                            ob = otk[n % 2]
                            obr = "otk%d" % (n % 2)
                            for hb in range(NHB):
                                pms = []
                                for kt_ in range(2):
                                    ps, pr = nps()
                                    mm_group(ps[:, 0:HB * 128].rearrange("p (h t) -> p h t", h=HB), pr,
                                             [(sk_t[b][:, (n + kt_) * 128:(n + kt_ + 1) * 128], sq_t[b][:, hb * HB:(hb + 1) * HB, n * 128:(n + 1) * 128])],
                                             ["sk%d" % b, "sq%d" % b])
                                    pi = pctr[0] % 4
                                    pctr[0] += 1
                                    op("scalar", lambda e, ps=ps, pi=pi: e.activation(out=Pe[pi][:, 0:HB * 128], in_=ps[:, 0:HB * 128], func=AF.Exp, scale=0.125),
                                       reads=[pr], writes=["Pe%d" % pi])
                                    mi = (2 if (n == 0 and seg == 0) else 0) if kt_ == 0 else 1
                                    op("gpsimd", lambda e, pi=pi, mi=mi: e.tensor_tensor(out=Pm[pi][:, 0:HB * 128], in0=Pe[pi][:, 0:HB * 128],
                                                                                       in1=msk[:, mi, 0:HB * 128], op=ALU.mult),
                                       reads=["Pe%d" % pi, "msk"], writes=["Pm%d" % pi])
                                    pms.append(pi)
                                pso, pro = nps()

                                def pvmm(e, pso=pso, pms=pms):
                                    for hl in range(HB):
                                        for kt_ in range(2):
                                            i = e.matmul(pso[:, hl * 65:(hl + 1) * 65], Pm[pms[kt_]][:, hl * 128:(hl + 1) * 128], sv_t[b][:, n + kt_, :],
                                                         start=(kt_ == 0), stop=(kt_ == 1))
                                    return i
                                op("tensor", pvmm, reads=["Pm%d" % pms[0], "Pm%d" % pms[1], "sv%d" % b], writes=[pro])
                                di = dctr[0] % 2
                                dctr[0] += 1
                                pv = pso[:, 0:HB * 65].rearrange("p (h d) -> p h d", h=HB)
                                hq0 = g * G + hb * HB
                                prs = [pro]
                                op("vector", lambda e, pv=pv, di=di, hq0=hq0: e.tensor_tensor(out=den[di][:, 0:HB], in0=pv[:, :, 64], in1=esk[:, hq0:hq0 + HB], op=ALU.add),
                                   reads=prs + ["esk"], writes=["den%d" % di])
                                op("vector", lambda e, di=di: e.reciprocal(out=rec[di][:, 0:HB], in_=den[di][:, 0:HB]), reads=["den%d" % di], writes=["rec%d" % di])
                                col = hb * HB * 64
                                op("vector", lambda e, pv=pv, di=di, col=col: e.tensor_tensor(
                                    out=ob[:, col:col + HB * 64].rearrange("p (h d) -> p h d", h=HB), in0=pv[:, :, 0:64],
                                    in1=rec[di][:, 0:HB].unsqueeze(2).to_broadcast([128, HB, 64]), op=ALU.mult),
                                   reads=prs + ["rec%d" % di], writes=[(obr, hb)])
                            grp = n // 4
                            sS = swS[(g * ((NCH + 3) // 4) + grp) % 2]
                            sSr = "swS%d" % ((g * ((NCH + 3) // 4) + grp) % 2)
                            pt, ptr = npb()

                            def tr3(e, pt=pt, ob=ob):
                                for j in range(NB):
                                    i = e.transpose(pt[0:min(128, G * 64), j * 128:(j + 1) * 128], ob[:, j * 128:j * 128 + min(128, G * 64)], idb[:])
                                return i
                            op("tensor", tr3, reads=[(obr, hb) for hb in range(NHB)] + ["idb"], writes=[ptr])
                            PW = min(128, G * 64)
                            for j in range(NB):
                                dst = sS[0:PW, j, (n % 4) * 128:(n % 4 + 1) * 128]
                                if False:
                                    pass
                                else:
                                    op("scalar", lambda e: e.activation(out=dst, in_=pt[0:PW, j * 128:(j + 1) * 128], func=AF.Copy), reads=[ptr], writes=[(sSr, n % 4, j)])
                            if n % 4 == 3 or n == NCH - 1:
                                ng = (n % 4) + 1
                                t0 = grp * 512
                                op("scalar", lambda e, sS=sS, t0=t0, ng=ng, g=g: e.dma_start(
                                    out=SWAT[g * G * 64:(g + 1) * G * 64, t0:t0 + ng * 128].rearrange("(c p) t -> p c t", p=PW),
                                    in_=sS[0:PW, 0:NB, 0:ng * 128]),
                                   reads=[(sSr, q, j) for q in range(ng) for j in range(NB)], writes=[("SWAT", g, grp)] + [(sSr, q, j) for q in range(ng) for j in range(NB)], dma=sSr)
                kb.barrier()

            if STOP >= 5 and not hist:
                def load_actT(ph_t, res_name, src, KC, tok0, ntok, src_res):
                    op("sync", lambda e: e.dma_start(out=ph_t[:, 0:KC, 0:ntok], in_=src[:, tok0:tok0 + ntok].rearrange("(c p) t -> p c t", p=128)),
                       reads=src_res, writes=[res_name], dma=res_name)

                with ExitStack() as ph:
                    KCm = max(RV, SQ, D) // 128
                    aT = sb(ph, "aT", [128, KCm, TR], BF16)
                    ws = WStream(ph, KCm, 512, 2, "m")
                    ld = [sb(ph, "ld", [128, 512]) for _ in range(4)]
                    st1 = [sb(ph, "st1", [128, 512]) for _ in range(3)]
                    stb1 = [sb(ph, "stb1", [128, 512], BF16) for _ in range(2)]
                    lctr = [0]

                    def nld():
                        i = lctr[0] % 4
                        lctr[0] += 1
                        return ld[i], "ld%d" % i
                    sc = [0, 0]
                    WCm = min(512, D)
                    for half in range(T // TR):
                        tok0 = half * TR
                        load_actT(aT, "aT", RETT, RV // 128, tok0, TR, [("RETT", gi) for gi in range((NCH + 3) // 4)])
                        for c0 in range(0, D, WCm):
                            wt, wtr = ws.load(Wro[0], Wro[1], RV // 128, c0, WCm)
                            for m in range(WCm // 128):
                                fi = (c0 // 128) + m
                                for tt in range(TR // TT):
                                    tsl = slice(tok0 + tt * TT, tok0 + (tt + 1) * TT)
                                    ps, pr = nps()
                                    mm_group(ps[:, 0:TT], pr, [(wt[:, k, m * 128:(m + 1) * 128], aT[:, k, tt * TT:(tt + 1) * TT]) for k in range(RV // 128)], ["aT", wtr])
                                    la, lar = nld()
                                    op("sync", lambda e, la=la, fi=fi, tsl=tsl: e.dma_start(out=la[:, 0:TT], in_=AR[fi * 128:(fi + 1) * 128, tsl]),
                                       reads=[("AR", fi, h2, t2) for h2 in range(T // TR) for t2 in range(TR // TT)], writes=[lar], dma=lar)
                                    i = sc[0] % 3
                                    sc[0] += 1
                                    so, sor = st1[i], "st1%d" % i
                                    op("vector", lambda e, ps=ps, la=la, so=so: e.tensor_tensor(out=so[:, 0:TT], in0=ps[:, 0:TT], in1=la[:, 0:TT], op=ALU.mult),
                                       reads=[pr, lar], writes=[sor])
                                    op("scalar", lambda e, so=so, fi=fi, tsl=tsl: e.dma_start(out=M1[fi * 128:(fi + 1) * 128, tsl], in_=so[:, 0:TT]),
                                       reads=[sor], writes=[("M1", fi, half, tt)], dma=sor)
                        load_actT(aT, "aT", SWAT, SQ // 128, tok0, TR, [("SWAT", g, gi) for g in range(HKV) for gi in range((NCH + 3) // 4)])
                        for c0 in range(0, D, WCm):
                            wt, wtr = ws.load(Wso[0], Wso[1], SQ // 128, c0, WCm)
                            for m in range(WCm // 128):
                                fi = (c0 // 128) + m
                                for tt in range(TR // TT):
                                    tsl = slice(tok0 + tt * TT, tok0 + (tt + 1) * TT)
                                    ps, pr = nps()
                                    mm_group(ps[:, 0:TT], pr, [(wt[:, k, m * 128:(m + 1) * 128], aT[:, k, tt * TT:(tt + 1) * TT]) for k in range(SQ // 128)], ["aT", wtr])
                                    la, lar = nld()
                                    op("sync", lambda e, la=la, fi=fi, tsl=tsl: e.dma_start(out=la[:, 0:TT], in_=AS[fi * 128:(fi + 1) * 128, tsl]),
                                       reads=[("AS", fi, h2, t2) for h2 in range(T // TR) for t2 in range(TR // TT)], writes=[lar], dma=lar)
                                    lm, lmr = nld()
                                    op("sync", lambda e, lm=lm, fi=fi, tsl=tsl: e.dma_start(out=lm[:, 0:TT], in_=M1[fi * 128:(fi + 1) * 128, tsl]),
                                       reads=[("M1", fi, half, tt)], writes=[lmr], dma=lmr)
                                    i = sc[0] % 3
                                    sc[0] += 1
                                    so, sor = st1[i], "st1%d" % i
                                    op("vector", lambda e, ps=ps, la=la, so=so: e.tensor_tensor(out=so[:, 0:TT], in0=ps[:, 0:TT], in1=la[:, 0:TT], op=ALU.mult),
                                       reads=[pr, lar], writes=[sor])
                                    j = sc[1] % 2
                                    sc[1] += 1
                                    sbo, sbr = stb1[j], "stb1%d" % j
                                    op("gpsimd", lambda e, so=so, lm=lm, sbo=sbo: e.tensor_tensor(out=sbo[:, 0:TT], in0=so[:, 0:TT], in1=lm[:, 0:TT], op=ALU.add),
                                       reads=[sor, lmr], writes=[sbr])
                                    op("scalar", lambda e, sbo=sbo, fi=fi, tsl=tsl: e.dma_start(out=MT[fi * 128:(fi + 1) * 128, tsl], in_=sbo[:, 0:TT]),
                                       reads=[sbr], writes=[("MT", fi, half, tt)], dma=sbr)
                        load_actT(aT, "aT", MT, KD, tok0, TR, [("MT", fi, half, tt) for fi in range(KD) for tt in range(TR // TT)])
                        for c0 in range(0, D, WCm):
                            wt, wtr = ws.load(Wmo[0], Wmo[1], KD, c0, WCm)
                            for tc in range(TR // 128):
                                rows = slice(tok0 + tc * 128, tok0 + (tc + 1) * 128)
                                ps, pr = nps()
                                mm_group(ps[:, 0:WCm], pr, [(aT[:, k, tc * 128:(tc + 1) * 128], wt[:, k, 0:WCm]) for k in range(KD)], ["aT", wtr])
                                la, lar = nld()
                                op("sync", lambda e, la=la, rows=rows, c0=c0: e.dma_start(out=la[:, 0:WCm], in_=x_in[rows, c0:c0 + WCm]), writes=[lar], dma=lar)
                                i = sc[0] % 3
                                sc[0] += 1
                                so, sor = st1[i], "st1%d" % i
                                op("vector", lambda e, ps=ps, la=la, so=so: e.tensor_tensor(out=so[:, 0:WCm], in0=ps[:, 0:WCm], in1=la[:, 0:WCm], op=ALU.add),
                                   reads=[pr, lar], writes=[sor])
                                op("scalar", lambda e, so=so, rows=rows, c0=c0: e.dma_start(out=X2[rows, c0:c0 + WCm], in_=so[:, 0:WCm]),
                                   reads=[sor], writes=[("X2", tok0 // 128 + tc, c0)], dma=sor)
                kb.barrier()

            if STOP >= 6 and not hist:
                XC = min(512, XW)
                TRx = min(T, 512)
                WCm = min(512, D)
                xsc = 1.0 / math.sqrt(256.0)
                with ExitStack() as ph6:
                    kxT = sb(ph6, "kxT", [128, XK, ML], BF16)
                    vx = sb(ph6, "vx", [128, ML // 128, XW], BF16)
                    with ExitStack() as ph:
                        gmem = load_gain(ph, g_mem, D, "gmem")
                        memT = sb(ph, "memT", [128, KD, ML], BF16)
                        ws = WStream(ph, KD, XC, 2, "xa")
                        nt = NormT(ph, "nm")
                        mkeys = nt.run(mem_in, ML // 128, gmem, "gmem", memT, "memT")
                        for c0 in range(0, 2 * XW, XC):
                            wt, wtr = ws.load(Wxkv[0], Wxkv[1], KD, c0, XC)
                            if c0 < XW:
                                for m in range(XC // 128):
                                    fi = c0 // 128 + m
                                    ps, pr = nps()
                                    mm_group(ps[:, 0:ML], pr, [(wt[:, k, m * 128:(m + 1) * 128], memT[:, k, :]) for k in range(KD)], mkeys + [wtr])
                                    op("vector", lambda e: e.tensor_copy(out=kxT[:, fi, :], in_=ps[:, 0:ML]), reads=[pr], writes=[("kxT", fi)])
                            else:
                                for mc in range(ML // 128):
                                    ps, pr = nps()
                                    mm_group(ps[:, 0:XC], pr, [(memT[:, k, mc * 128:(mc + 1) * 128], wt[:, k, 0:XC]) for k in range(KD)], mkeys + [wtr])
                                    cc = c0 - XW
                                    op("vector", lambda e: e.tensor_copy(out=vx[:, mc, cc:cc + XC], in_=ps[:, 0:XC]), reads=[pr], writes=[("vx", mc, cc)])
                    kb.barrier()
                    with ExitStack() as ph:
                        gxat = load_gain(ph, g_xat, D, "gxat")
                        hxT = sb(ph, "hxT", [128, KD, TRx], BF16)
                        qxT = sb(ph, "qxT", [128, XK, TRx], BF16)
                        oxT = sb(ph, "oxT", [128, XK, TRx], BF16)
                        wsq = WStream(ph, KD, 256, 2, "xq")
                        wso = WStream(ph, XK, WCm, 2, "xo")
                        Px = [sb(ph, "Px", [128, TT], BF16) for _ in range(4)]
                        rcx = [sb(ph, "rcx", [128, TT]) for _ in range(2)]
                        ld = [sb(ph, "ldx", [128, 512]) for _ in range(2)]
                        st1 = [sb(ph, "stx", [128, 512]) for _ in range(2)]
                        nt = NormT(ph, "nx")
                        pxc = [0]
                        XQC = min(256, XW)
                        for part in range(T // TRx):
                            tok0 = part * TRx
                            hkeys = nt.run(X2[tok0:tok0 + TRx, :], TRx // 128, gxat, "gxat", hxT, "hxT")
                            for c0 in range(0, XW, XQC):
                                wt, wtr = wsq.load(Wxq[0], Wxq[1], KD, c0, XQC)
                                for m in range(XQC // 128):
                                    fi = c0 // 128 + m
                                    for tt in range(TRx // TT):
                                        ps, pr = nps()
                                        mm_group(ps[:, 0:TT], pr, [(wt[:, k, m * 128:(m + 1) * 128], hxT[:, k, tt * TT:(tt + 1) * TT]) for k in range(KD)], hkeys + [wtr])
                                        op("scalar", lambda e: e.activation(out=qxT[:, fi, tt * TT:(tt + 1) * TT], in_=ps[:, 0:TT], func=AF.Copy),
                                           reads=[pr], writes=[("qxT", fi, tt)])
                            for xh in range(XH):
                                for tt in range(TRx // TT):
                                    pis = []
                                    for mt in range(ML // 128):
                                        ps, pr = nps()
                                        mm_group(ps[:, 0:TT], pr, [(kxT[:, xh * 2 + dt, mt * 128:(mt + 1) * 128], qxT[:, xh * 2 + dt, tt * TT:(tt + 1) * TT]) for dt in range(2)],
                                                 [("qxT", xh * 2 + dt, tt) for dt in range(2)])
                                        pi = pxc[0] % 4
                                        pxc[0] += 1
                                        op("scalar", lambda e: e.activation(out=Px[pi][:], in_=ps[:, 0:TT], func=AF.Exp, scale=xsc), reads=[pr], writes=["Px%d" % pi])
                                        pis.append(pi)
                                    psd, prd = nps()
                                    mm_group(psd[:, 0:TT], prd, [(ones[:], Px[pi][:]) for pi in pis], ["Px%d" % pi for pi in pis])
                                    ri = (xh * (TRx // TT) + tt) % 2
                                    op("vector", lambda e: e.reciprocal(out=rcx[ri][:], in_=psd[:, 0:TT]), reads=[prd], writes=["rcx%d" % ri])
                                    for dvt in range(2):
                                        pso, pro = nps()
                                        mm_group(pso[:, 0:TT], pro, [(vx[:, mt, xh * 256 + dvt * 128:xh * 256 + (dvt + 1) * 128], Px[pis[mt]][:]) for mt in range(ML // 128)],
                                                 ["Px%d" % pi for pi in pis])
                                        op("vector", lambda e: e.tensor_tensor(out=oxT[:, xh * 2 + dvt, tt * TT:(tt + 1) * TT], in0=pso[:, 0:TT], in1=rcx[ri][:], op=ALU.mult),
                                           reads=[pro, "rcx%d" % ri], writes=[("oxT", xh * 2 + dvt, tt)])
                            okeys = [("oxT", f, tt) for f in range(XK) for tt in range(TRx // TT)]
                            for c0 in range(0, D, WCm):
                                wt, wtr = wso.load(Wxo[0], Wxo[1], XK, c0, WCm)
                                for tc in range(TRx // 128):
                                    rows = slice(tok0 + tc * 128, tok0 + (tc + 1) * 128)
                                    ps, pr = nps()
                                    mm_group(ps[:, 0:WCm], pr, [(oxT[:, k, tc * 128:(tc + 1) * 128], wt[:, k, 0:WCm]) for k in range(XK)], okeys + [wtr])
                                    li = (c0 // WCm + tc) % 2
                                    la, lar = ld[li], "ldx%d" % li
                                    op("sync", lambda e: e.dma_start(out=la[:, 0:WCm], in_=X2[rows, c0:c0 + WCm]), writes=[lar], dma=lar)
                                    so, sor = st1[li], "stx%d" % li
                                    op("vector", lambda e: e.tensor_tensor(out=so[:, 0:WCm], in0=ps[:, 0:WCm], in1=la[:, 0:WCm], op=ALU.add),
                                       reads=[pr, lar], writes=[sor])
                                    op("scalar", lambda e: e.dma_start(out=X3[rows, c0:c0 + WCm], in_=so[:, 0:WCm]), reads=[sor], writes=[sor + "d"], dma=sor)
                kb.barrier()

            if STOP >= 7 and not hist:
                BIG = 1.0e9
                with ExitStack() as ph:
                    gmoe = load_gain(ph, g_moe, D, "gmoe")
                    brt = load_gain(ph, b_r, NR, "brt")
                    wr = sb(ph, "wr", [128, KD, NR], BF16)
                    op("sync", lambda e: e.dma_start(out=wr[:], in_=Wr[0].rearrange("(c p) n -> p c n", p=128)), writes=["wr"], dma="const")
                    op("sync", lambda e: e.nop(), reads=["wr"])
                    xnq = sb(ph, "xnq", [128, KD, TQ], BF16)
                    WT = sb(ph, "WT", [NE, TQ])
                    nt = NormT(ph, "nq")
                    names = ["lg", "mxg", "g1h", "sh", "eg", "sg1", "gtop", "pen", "le", "m1", "k1", "le2", "m2", "k2", "dm", "ed", "e1", "r1", "w1", "w2", "wa", "wf"]
                    widths = dict(lg=NR, g1h=NG, sh=NG, eg=NG, pen=NG, le=NE, k1=NE, le2=NE, k2=NE, wa=NE, wf=NE)
                    tl = {nm: sb(ph, nm, [128, widths.get(nm, 1)]) for nm in names}

                    def bc(t, n):
                        return t[:, 0:1].to_broadcast([128, n])
                    V = "vector"
                    for q in range(T // TQ):
                        tok0 = q * TQ
                        xkeys = nt.run(X3[tok0:tok0 + TQ, :], TQ // 128, gmoe, "gmoe", xnq, "xnq")
                        op("scalar", lambda e: e.dma_start(out=XNT[:, tok0:tok0 + TQ].rearrange("(c p) t -> p c t", p=128), in_=xnq[:]),
                           reads=xkeys, writes=xkeys, dma="xnq")
                        for tc in range(TQ // 128):
                            ps, pr = nps()
                            mm_group(ps[:, 0:NR], pr, [(xnq[:, k, tc * 128:(tc + 1) * 128], wr[:, k, :]) for k in range(KD)], xkeys + ["wr"])
                            lg, mxg, g1h, sh, eg, sg1, gtop, pen, le, m1 = [tl[n_] for n_ in names[:10]]
                            k1, le2, m2, k2, dm, ed, e1, r1, w1, w2, wa, wf = [tl[n_] for n_ in names[10:]]
                            op(V, lambda e: e.tensor_tensor(out=lg[:], in0=ps[:, 0:NR], in1=brt[:], op=ALU.add), reads=[pr, "brt"], writes=["lg"])
                            op(V, lambda e: e.reduce_max(out=mxg[:], in_=lg[:, 0:NG], axis=AX.X), reads=["lg"], writes=["mxg"])
                            op(V, lambda e: e.tensor_tensor(out=g1h[:], in0=lg[:, 0:NG], in1=bc(mxg, NG), op=ALU.is_equal), reads=["lg", "mxg"], writes=["g1h"])
                            op(V, lambda e: e.tensor_tensor(out=sh[:], in0=lg[:, 0:NG], in1=bc(mxg, NG), op=ALU.subtract), reads=["lg", "mxg"], writes=["sh"])
                            op("scalar", lambda e: e.activation(out=eg[:], in_=sh[:], func=AF.Exp, accum_out=sg1[:]), reads=["sh"], writes=["eg", "sg1"])
                            op(V, lambda e: e.reciprocal(out=gtop[:], in_=sg1[:]), reads=["sg1"], writes=["gtop"])
                            op(V, lambda e: e.tensor_scalar(out=pen[:], in0=g1h[:], scalar1=-1.0, scalar2=BIG, op0=ALU.add, op1=ALU.mult), reads=["g1h"], writes=["pen"])
                            op(V, lambda e: e.tensor_tensor(out=le[:].rearrange("p (g x) -> p g x", g=NG), in0=lg[:, NG:NR].rearrange("p (g x) -> p g x", g=NG),
                                                            in1=pen[:].unsqueeze(2).to_broadcast([128, NG, EPG]), op=ALU.add), reads=["lg", "pen"], writes=["le"])
                            op(V, lambda e: e.reduce_max(out=m1[:], in_=le[:], axis=AX.X), reads=["le"], writes=["m1"])
                            op(V, lambda e: e.tensor_tensor(out=k1[:], in0=le[:], in1=bc(m1, NE), op=ALU.is_equal), reads=["le", "m1"], writes=["k1"])
                            op(V, lambda e: e.scalar_tensor_tensor(out=le2[:], in0=k1[:], scalar=-BIG, in1=le[:], op0=ALU.mult, op1=ALU.add), reads=["k1", "le"], writes=["le2"])
                            op(V, lambda e: e.reduce_max(out=m2[:], in_=le2[:], axis=AX.X), reads=["le2"], writes=["m2"])
                            op(V, lambda e: e.tensor_tensor(out=k2[:], in0=le2[:], in1=bc(m2, NE), op=ALU.is_equal), reads=["le2", "m2"], writes=["k2"])
                            op(V, lambda e: e.tensor_tensor(out=dm[:], in0=m2[:], in1=m1[:], op=ALU.subtract), reads=["m1", "m2"], writes=["dm"])
                            op("scalar", lambda e: e.activation(out=ed[:], in_=dm[:], func=AF.Exp), reads=["dm"], writes=["ed"])
                            op(V, lambda e: e.tensor_scalar(out=e1[:], in0=ed[:], scalar1=1.0, scalar2=1.0, op0=ALU.add, op1=ALU.mult), reads=["ed"], writes=["e1"])
                            op(V, lambda e: e.reciprocal(out=r1[:], in_=e1[:]), reads=["e1"], writes=["r1"])
                            op(V, lambda e: e.tensor_tensor(out=w1[:], in0=r1[:], in1=gtop[:], op=ALU.mult), reads=["r1", "gtop"], writes=["w1"])
                            op(V, lambda e: e.tensor_tensor(out=w2[:], in0=w1[:], in1=ed[:], op=ALU.mult), reads=["w1", "ed"], writes=["w2"])
                            op(V, lambda e: e.tensor_tensor(out=wa[:], in0=k1[:], in1=bc(w1, NE), op=ALU.mult), reads=["k1", "w1"], writes=["wa"])
                            op(V, lambda e: e.scalar_tensor_tensor(out=wf[:], in0=k2[:], scalar=w2[:, 0:1], in1=wa[:], op0=ALU.mult, op1=ALU.add), reads=["k2", "w2", "wa"], writes=["wf"])
                            pst, prt = nps()
                            op("tensor", lambda e: e.transpose(pst[0:NE, 0:128], wf[:], idf[:]), reads=["wf", "idf"], writes=[prt])
                            op(V, lambda e: e.tensor_copy(out=WT[:, tc * 128:(tc + 1) * 128], in_=pst[0:NE, 0:128]), reads=[prt], writes=[("WT", tc)])
                        wtk = [("WT", tc) for tc in range(TQ // 128)]
                        op("sync", lambda e: e.dma_start(out=WTD[q, :, :], in_=WT[:]), reads=wtk, writes=wtk, dma="WT")
                kb.barrier()

            if STOP >= 8 and not hist:
                with ExitStack() as ph:
                    xnT = sb(ph, "xnT", [128, KD, TQ], BF16)
                    yacc = sb(ph, "yacc", [128, KD, TQ])
                    gcol = 256 if DE >= 256 else DE
                    NGH = DE // gcol
                    HPG = gcol // 128
                    wgu = [sb(ph, "wgu", [128, KD, gcol], BF16) for _ in range(3)]
                    wdt = sb(ph, "wdt", [128, HT, D], BF16)
                    sgs = sb(ph, "sgs", [128, HT, TQ])
                    hid = sb(ph, "hid", [128, HT, TQ], BF16)
                    wbt = [sb(ph, "wbt", [128, TQ]) for _ in range(2)]
                    tmpu = [sb(ph, "tmpu", [128, TQ]) for _ in range(2)]
                    gctr = [0]
                    for q in range(T // TQ):
                        tok0 = q * TQ
                        op("sync", lambda e: e.dma_start(out=xnT[:], in_=XNT[:, tok0:tok0 + TQ].rearrange("(c p) t -> p c t", p=128)), writes=["xnT"], dma="xnT")
                        for ex in range(NE):
                            rk, j = ex // EL, ex % EL
                            wi = ex % 2
                            op("sync", lambda e: e.dma_start(out=wbt[wi][:], in_=WTD[q, ex:ex + 1, :].partition_broadcast(128)), writes=["wbt%d" % wi], dma="wbt%d" % wi)
                            op("sync", lambda e: e.dma_start(out=wdt[:], in_=Wd[j][0][rk * DE:(rk + 1) * DE, :].rearrange("(c p) n -> p c n", p=128)),
                               reads=[Wd[j][1]], writes=["wdt"], dma="wdt")
                            for which in range(2):
                                Wsrc = (Wg if which == 0 else Wu)[j]
                                for gh in range(NGH):
                                    gi = gctr[0] % 3
                                    gctr[0] += 1
                                    wtile, wres = wgu[gi], "wgu%d" % gi
                                    op("sync", lambda e: e.dma_start(
                                        out=wtile[:], in_=Wsrc[0][rk * D:(rk + 1) * D, gh * gcol:(gh + 1) * gcol].rearrange("(c p) n -> p c n", p=128)),
                                       reads=[Wsrc[1]], writes=[wres], dma=wres)
                                    for hl in range(HPG):
                                        ht = gh * HPG + hl
                                        ps, pr = nps()
                                        mm_group(ps[:, 0:TQ], pr, [(wtile[:, k, hl * 128:(hl + 1) * 128], xnT[:, k, :]) for k in range(KD)], ["xnT", wres])
                                        if which == 0:
                                            op("scalar", lambda e: e.activation(out=sgs[:, ht, :], in_=ps[:, 0:TQ], func=AF.Silu), reads=[pr], writes=[("sgs", ht)])
                                        else:
                                            ti = ht % 2
                                            op("vector", lambda e: e.tensor_tensor(out=tmpu[ti][:], in0=sgs[:, ht, :], in1=ps[:, 0:TQ], op=ALU.mult),
                                               reads=[pr, ("sgs", ht)], writes=["tmpu%d" % ti])
                                            op("gpsimd", lambda e: e.tensor_tensor(out=hid[:, ht, :], in0=tmpu[ti][:], in1=wbt[wi][:], op=ALU.mult),
                                               reads=["tmpu%d" % ti, "wbt%d" % wi], writes=[("hid", ht)])
                            for f in range(KD):
                                ps, pr = nps()
                                mm_group(ps[:, 0:TQ], pr, [(wdt[:, ht, f * 128:(f + 1) * 128], hid[:, ht, :]) for ht in range(HT)], [("hid", ht) for ht in range(HT)] + ["wdt"])
                                if ex == 0:
                                    op("vector", lambda e: e.tensor_copy(out=yacc[:, f, :], in_=ps[:, 0:TQ]), reads=[pr], writes=[("yacc", f)])
                                else:
                                    op("vector", lambda e: e.tensor_tensor(out=yacc[:, f, :], in0=yacc[:, f, :], in1=ps[:, 0:TQ], op=ALU.add),
                                       reads=[pr, ("yacc", f)], writes=[("yacc", f)])
                        yk = [("yacc", f) for f in range(KD)]
                        op("scalar", lambda e: e.dma_start(out=YT[:, tok0:tok0 + TQ].rearrange("(c p) t -> p c t", p=128), in_=yacc[:]), reads=yk, writes=yk, dma="yacc")
                kb.barrier()

            if STOP >= 9 and not hist:
                with ExitStack() as ph:
                    gfin = load_gain(ph, g_fin, D, "gfin")
                    ytl = [sb(ph, "ytl", [128, KD, 128]) for _ in range(2)]
                    xo = [sb(ph, "xo", [128, D]) for _ in range(2)]
                    xs = [sb(ph, "xs", [128, D]) for _ in range(2)]
                    jb = sb(ph, "jb", [128, D], BF16)
                    yo = [sb(ph, "yo", [128, D]) for _ in range(2)]
                    fss = sb(ph, "fss", [128, 1]); fms = sb(ph, "fms", [128, 1]); fsd = sb(ph, "fsd", [128, 1]); frs = sb(ph, "frs", [128, 1])
                    for n in range(NCH):
                        b = n % 2
                        rows = slice(n * 128, (n + 1) * 128)
                        op("sync", lambda e: e.dma_start(out=xo[b][:], in_=X3[rows, :]), writes=["xo%d" % b], dma="xo%d" % b)
                        op("sync", lambda e: e.dma_start(out=ytl[b][:], in_=YT[:, n * 128:(n + 1) * 128].rearrange("(c p) t -> p c t", p=128)), writes=["ytl%d" % b], dma="ytl%d" % b)
                        for f4 in range(0, KD, 4):
                            nk = min(4, KD - f4)
                            ps, pr = nps()

                            def tr4(e):
                                for jj in range(nk):
                                    i = e.transpose(ps[:, jj * 128:(jj + 1) * 128], ytl[b][:, f4 + jj, :], idf[:])
                                return i
                            op("tensor", tr4, reads=["ytl%d" % b, "idf"], writes=[pr])
                            op("vector", lambda e: e.tensor_tensor(out=xs[b][:, f4 * 128:(f4 + nk) * 128], in0=xo[b][:, f4 * 128:(f4 + nk) * 128], in1=ps[:, 0:nk * 128], op=ALU.add),
                               reads=[pr, "xo%d" % b], writes=[("xs%d" % b, f4)])
                        xsk = [("xs%d" % b, f4) for f4 in range(0, KD, 4)]
                        op("scalar", lambda e: e.activation(out=jb[:], in_=xs[b][:], func=AF.Square, accum_out=fss[:]), reads=xsk, writes=["jb", "fss"])
                        op("vector", lambda e: e.tensor_scalar(out=fms[:], in0=fss[:], scalar1=1.0 / D, scalar2=RMS_EPS, op0=ALU.mult, op1=ALU.add), reads=["fss"], writes=["fms"])
                        op("scalar", lambda e: e.activation(out=fsd[:], in_=fms[:], func=AF.Sqrt), reads=["fms"], writes=["fsd"])
                        op("vector", lambda e: e.reciprocal(out=frs[:], in_=fsd[:]), reads=["fsd"], writes=["frs"])
                        op("vector", lambda e: e.scalar_tensor_tensor(out=yo[b][:], in0=xs[b][:], scalar=frs[:, 0:1], in1=gfin[:], op0=ALU.mult, op1=ALU.mult),
                           reads=xsk + ["frs", "gfin"], writes=["yo%d" % b])
                        op("sync", lambda e: e.dma_start(out=y_out[rows, :], in_=yo[b][:]), reads=["yo%d" % b], writes=["yo%d" % b], dma="yo%d" % b)
                kb.barrier()
        print("bass ops:", kb.nops, "sems:", len(kb.sems), "max sem value:", max(kb.semval.values()))
    return nc


def host_inputs(c, inp):
    f32 = np.float32
    bf = ml_dtypes.bfloat16
    D, T, H, NCH = c.D, c.T, c.H, c.NCH
    x = np.asarray(inp["x"], f32)
    mem = np.asarray(inp["mem"], f32)
    w_in = np.asarray(inp["w_in"], f32)[0]
    w_r = np.concatenate([np.asarray(inp["w_router_group"], f32)[0], np.asarray(inp["w_router_expert"], f32)[0]], axis=1)
    b_r = np.concatenate([np.asarray(inp["b_router_group"], f32)[0], np.asarray(inp["b_router_expert"], f32)[0]])[None, :]
    wg = np.asarray(inp["w_exp_gate"], f32)[0].reshape(c.NE * D, c.DE)
    wu = np.asarray(inp["w_exp_up"], f32)[0].reshape(c.NE * D, c.DE)
    wd = np.asarray(inp["w_exp_down"], f32)[0].reshape(c.NE * c.DE, D)
    hh = np.arange(H, dtype=f32)
    log_gamma = np.log1p(-np.exp2(-5.0 - hh)).astype(f32)
    idx = np.arange(128, dtype=f32)
    diff = idx[None, :] - idx[:, None]
    dec = np.where(diff[:, None, :] >= 0, np.exp(log_gamma[None, :, None] * np.maximum(diff, 0.0)[:, None, :]), 0.0).astype(f32)
    qkd = np.zeros((128, 3, H), f32)
    qkd[:, 0, :] = np.exp(log_gamma[None, :] * (idx + 1.0)[:, None])
    qkd[:, 1, :] = np.exp(log_gamma[None, :] * (128 - 1.0 - idx)[:, None])
    qkd[:, 2, :] = np.exp(log_gamma * 128.0)[None, :]
    half = 64
    inv = (ROPE_BASE ** (-np.arange(half, dtype=f32) / half)).astype(f32)
    qi = np.arange(128)[None, :]
    kj = np.arange(128)[:, None]
    m_prev = (kj > qi).astype(f32)
    m_cur = (kj <= qi).astype(f32)
    idb = np.eye(128, dtype=f32).astype(bf)
    idf = np.eye(128, dtype=f32)
    ones = np.ones((128, 128), f32).astype(bf)
    maps = []
    NCORES, NSEG, NW = c.NC, c.NS, c.NW
    for core in range(NCORES):
        b, s = core // NSEG, core % NSEG
        t0 = s * T
        NSG = c.NSG
        TS = T * NSG
        t0 = s * TS
        pos = (t0 + np.arange(TS)).astype(f32)
        ang = pos[:, None] * inv[None, :]
        cosv, sinv = np.cos(ang).astype(f32), np.sin(ang).astype(f32)
        ks = f32(128.0 ** -0.5)
        cs = np.stack([cosv, sinv, cosv * ks, sinv * ks], axis=1)
        cs = cs.reshape(NSG, NCH, 128, 4, 64).transpose(0, 2, 1, 3, 4).copy()
        coef = np.zeros((128, 8, H), f32)
        for j in range(NCORES if NSEG > 1 else 0):
            bj, sj = j // NSEG, j % NSEG
            if bj == b and sj < s:
                coef[:, j, :] = np.exp(log_gamma * f32(TS * (s - 1 - sj)))[None, :]
        mp0 = m_prev if s > 0 else np.zeros_like(m_prev)
        msk = np.stack([np.tile(m_prev, (1, 4)), np.tile(m_cur, (1, 4)), np.tile(mp0, (1, 4))], axis=1).astype(bf)
        NHT = max(c.NHS, 1) * T
        xhist = np.zeros((NHT, D), f32)
        if c.NHS > 0 and s > 0:
            xhist[NHT - t0:] = x[b, 0:t0]
        posh = ((t0 - NHT) + np.arange(NHT)).astype(f32)
        angh = posh[:, None] * inv[None, :]
        ch, sh_ = np.cos(angh).astype(f32), np.sin(angh).astype(f32)
        cs_h = np.stack([ch, sh_, ch * ks, sh_ * ks], axis=1).reshape(max(c.NHS, 1), NCH, 128, 4, 64).transpose(0, 2, 1, 3, 4).copy()
        xh = np.zeros((NSG, 128, D), f32)
        for sg_ in range(NSG):
            if t0 + sg_ * T > 0:
                xh[sg_] = x[b, t0 + sg_ * T - 128:t0 + sg_ * T]
        r = core if NW > 1 else 0
        m = {
            "x": x[b, t0:t0 + TS], "xh": xh, "xhist": xhist, "cs_h": cs_h, "mem": mem[b],
            "g_mix": np.asarray(inp["mix_norm_g"], f32)[0][None, :], "g_xat": np.asarray(inp["xattn_norm_g"], f32)[0][None, :],
            "g_mem": np.asarray(inp["mem_norm_g"], f32)[0][None, :], "g_moe": np.asarray(inp["moe_norm_g"], f32)[0][None, :],
            "g_fin": np.asarray(inp["final_norm_g"], f32)[None, :], "g_ret": np.asarray(inp["ret_norm_g"], f32)[0].reshape(1, -1),
            "sinks": np.asarray(inp["swa_sinks"], f32)[0][None, :], "b_r": b_r,
            "w_in": w_in[r * (D // NW):(r + 1) * (D // NW)],
            "w_ro": np.asarray(inp["w_ret_o"], f32)[0][r * (c.RV // NW):(r + 1) * (c.RV // NW)],
            "w_so": np.asarray(inp["w_swa_o"], f32)[0][r * (c.SQ // NW):(r + 1) * (c.SQ // NW)],
            "w_mo": np.asarray(inp["w_mix_o"], f32)[0][r * (D // NW):(r + 1) * (D // NW)],
            "w_xq": np.asarray(inp["w_xq"], f32)[0][r * (D // NW):(r + 1) * (D // NW)],
            "w_xkv": np.asarray(inp["w_xkv"], f32)[0][r * (D // NW):(r + 1) * (D // NW)],
            "w_xo": np.asarray(inp["w_xo"], f32)[0][r * (c.XW // NW):(r + 1) * (c.XW // NW)],
            "w_r": w_r[r * (D // NW):(r + 1) * (D // NW)],
            "w_g": wg[r * c.EL * D:(r + 1) * c.EL * D], "w_u": wu[r * c.EL * D:(r + 1) * c.EL * D],
            "w_d": wd[r * c.EL * c.DE:(r + 1) * c.EL * c.DE],
            "cs": cs, "dec": dec, "qkd": qkd, "coef": coef, "msk": msk, "idb": idb, "idf": idf, "ones": ones,
        }
        import os
        if os.environ.get('KNOAG'):
            full = {"in": w_in, "ro": np.asarray(inp["w_ret_o"], f32)[0], "so": np.asarray(inp["w_swa_o"], f32)[0], "mo": np.asarray(inp["w_mix_o"], f32)[0],
                    "xkv": np.asarray(inp["w_xkv"], f32)[0], "xq": np.asarray(inp["w_xq"], f32)[0], "xo": np.asarray(inp["w_xo"], f32)[0], "r": w_r}
            for nm_, wfull in full.items():
                if nm_ == "in":
                    for ci, c0 in enumerate(range(0, c.INW, c.CW)):
                        m["dbgw_in_%d" % ci] = wfull[:, c0:c0 + c.CW].astype(bf)
                else:
                    m["dbgw_%s_0" % nm_] = wfull.astype(bf)
            for nm_, wfull, rr in (("g", wg, D), ("u", wu, D), ("d", wd, c.DE)):
                w4 = wfull.reshape(NW, c.EL, rr, -1)
                for j in range(c.EL):
                    m["dbgw_%s_%d" % (nm_, j)] = w4[:, j].reshape(NW * rr, -1).astype(bf)
            for kk in ["w_in", "w_ro", "w_so", "w_mo", "w_xq", "w_xkv", "w_xo", "w_r", "w_g", "w_u", "w_d"]:
                pass
        maps.append({k: np.ascontiguousarray(v) for k, v in m.items()})
    return maps


def run(c, inp):
    nc = build(c)
    maps = host_inputs(c, inp)
    NCORES, NSEG = c.NC, c.NS
    res = run_bass_kernel_spmd(nc, maps, core_ids=list(range(NCORES)))
    out = np.zeros((c.B, c.SEQ, c.D), np.float32)
    TS = c.T * c.NSG
    for core in range(NCORES):
        b, s = core // NSEG, core % NSEG
        out[b, s * TS:(s + 1) * TS] = res.results[core]["y"]
    return out


def kernel(**inputs):
    return run(Cfg(**FULL), inputs)
```

```python
import math
from contextlib import ExitStack
import numpy as np
import ml_dtypes
import concourse.bass as bass
import concourse.mybir as mybir
from concourse.bass_utils import run_bass_kernel_spmd

F32 = mybir.dt.float32
BF16 = mybir.dt.bfloat16
AF = mybir.ActivationFunctionType
ALU = mybir.AluOpType
AX = mybir.AxisListType
ENGS = ["tensor", "vector", "scalar", "gpsimd", "sync"]
EPOCH = 30000
RMS_EPS = 1e-6
ROPE_BASE = 10000.0

FULL = dict(D=4096, B=2, SEQ=8192, H=16, HQ=64, HKV=8, XH=4, ML=256, NG=8, EPG=8, DE=512, NC=4)


class Cfg:
    def __init__(self, **kw):
        self.__dict__.update(kw)
        c = self
        c.NC = getattr(c, 'NC', 8)
        c.NS = c.NC // c.B
        c.NW = c.NC if c.NC == 8 else 1
        c.NSG = getattr(c, 'NSG', {8: 1, 4: 2, 2: 4}[c.NC])
        c.T = c.SEQ // (c.NS * c.NSG)
        c.NHS = (c.NS - 1) * c.NSG if c.NW == 1 else 0
        c.NCH = c.T // 128
        c.KD = c.D // 128
        c.QK = c.H * 128
        c.RV = c.H * 256
        c.SQ = c.HQ * 64
        c.SKV = c.HKV * 64
        c.G = c.HQ // c.HKV
        c.XW = c.XH * 256
        c.NE = c.NG * c.EPG
        c.NR = c.NG + c.NE
        c.EL = c.NE // c.NW
        c.widths = [c.QK, c.QK, c.RV, c.RV, c.SQ, c.SKV, c.SKV, c.D, c.D]
        c.offs = [0] + [int(v) for v in np.cumsum(c.widths)]
        c.INW = c.offs[-1]
        c.CW = 5120 if c.INW % 5120 == 0 else c.INW
        c.TR = min(c.T, getattr(c, 'TRmax', 1024))
        c.TT = min(c.T, getattr(c, 'TTmax', 512))
        c.TQ = min(c.T, getattr(c, 'TQmax', 512))


class KB:
    def __init__(self, nc, stack):
        self.nc = nc
        self.stack = stack
        self.count = {e: 0 for e in ENGS}
        self.waited = {e: {} for e in ENGS}
        self.res = {}
        self.sems = {}
        self.semval = {}
        self.nops = 0

    def sem(self, key):
        if key not in self.sems:
            self.sems[key] = self.stack.enter_context(self.nc.semaphore("s%d" % len(self.sems)))
        return self.sems[key]

    def op(self, eng, fn, reads=(), writes=(), dma=None, dma_inc=16):
        need = {}
        for r in reads:
            st = self.res.get(r)
            if st:
                for s, v in st[0].items():
                    if need.get(s, 0) < v:
                        need[s] = v
        for w in writes:
            st = self.res.get(w)
            if st:
                for d in st:
                    for s, v in d.items():
                        if need.get(s, 0) < v:
                            need[s] = v
        h = getattr(self.nc, eng)
        wt = self.waited[eng]
        for s, v in need.items():
            if wt.get(s, 0) < v:
                wt[s] = v
                h.wait_ge(self.sems[s], v)
        if dma is None:
            c = self.count[eng]
            self.count[eng] = c + 1
            ev = ("%s%d" % (eng, c // EPOCH), c % EPOCH + 1)
            inc = 1
        else:
            k = "dma_" + dma
            ev = (k, self.semval.get(k, 0) + dma_inc)
            inc = dma_inc
        self.semval[ev[0]] = ev[1]
        ins = fn(h)
        ins.then_inc(self.sem(ev[0]), inc)
        self.nops += 1
        for r in reads:
            st = self.res.setdefault(r, ({}, {}))
            if st[1].get(ev[0], 0) < ev[1]:
                st[1][ev[0]] = ev[1]
        for w in writes:
            self.res[w] = ({ev[0]: ev[1]}, {})
        return ev

    def barrier(self):
        for e in ENGS:
            h = getattr(self.nc, e)
            wt = self.waited[e]
            for s, v in self.semval.items():
                if wt.get(s, 0) < v:
                    wt[s] = v
                    h.wait_ge(self.sems[s], v)
        self.res = {}


def build(c, dbg=()):
    import os
    STOP = int(os.environ.get('KSTOP', '99'))
    NOAG = bool(os.environ.get('KNOAG'))
    nc = bass.Bass("TRN2", target_bir_lowering=False)
    D, T, NCH, KD, H = c.D, c.T, c.NCH, c.KD, c.H
    QK, RV, SQ, SKV, G, HQ, HKV = c.QK, c.RV, c.SQ, c.SKV, c.G, c.HQ, c.HKV
    XW, XH, ML, NE, NR, NG, EPG, DE, EL = c.XW, c.XH, c.ML, c.NE, c.NR, c.NG, c.EPG, c.DE, c.EL
    NW, NS, NCORES = c.NW, c.NS, c.NC
    TR, TT, TQ = c.TR, c.TT, c.TQ
    XK = XW // 128
    HT = DE // 128

    def din(name, shape, dt=F32):
        return nc.dram_tensor(name, list(shape), dt, kind="ExternalInput").ap()

    def dscr(name, shape, dt):
        return nc.dram_tensor(name, list(shape), dt, kind="Internal").ap()

    NSG = c.NSG
    x_all = din("x", [NSG * T, D])
    xh_all = din("xh", [NSG, 128, D])
    NHS = c.NHS
    xhist = din("xhist", [max(NHS, 1) * T, D])
    cs_hist = din("cs_h", [max(NHS, 1), 128, NCH, 4, 64])
    mem_in = din("mem", [ML, D])
    g_mix = din("g_mix", [1, D]); g_xat = din("g_xat", [1, D]); g_mem = din("g_mem", [1, D])
    g_moe = din("g_moe", [1, D]); g_fin = din("g_fin", [1, D]); g_ret = din("g_ret", [1, RV])
    sinks_in = din("sinks", [1, HQ]); b_r = din("b_r", [1, NR])
    w_in_sh = din("w_in", [D // NW, c.INW]); w_ro_sh = din("w_ro", [RV // NW, D]); w_so_sh = din("w_so", [SQ // NW, D])
    w_mo_sh = din("w_mo", [D // NW, D]); w_xq_sh = din("w_xq", [D // NW, XW]); w_xkv_sh = din("w_xkv", [D // NW, 2 * XW])
    w_xo_sh = din("w_xo", [XW // NW, D]); w_r_sh = din("w_r", [D // NW, NR])
    w_g_sh = din("w_g", [EL * D, DE]); w_u_sh = din("w_u", [EL * D, DE]); w_d_sh = din("w_d", [EL * DE, D])
    cs_all = din("cs", [NSG, 128, NCH, 4, 64]); dec_in = din("dec", [128, H, 128]); qkd_in = din("qkd", [128, 3, H])
    coef_in = din("coef", [128, 8, H]); msk_in = din("msk", [128, 3, 512], BF16)
    idb_in = din("idb", [128, 128], BF16); idf_in = din("idf", [128, 128]); ones_in = din("ones", [128, 128], BF16)
    y_all = nc.dram_tensor("y", [NSG * T, D], F32, kind="ExternalOutput").ap()
    RST = dscr("RST", [128, H * 256], F32)

    RQ = dscr("RQ", [T, QK], BF16); RK = dscr("RK", [T, QK], BF16); RVs = dscr("RVs", [T, RV], BF16)
    RG = dscr("RG", [T, RV], F32)
    SQT = dscr("SQT", [HQ, 64, T], BF16); SKT = dscr("SKT", [HKV, 64, 128 + T], BF16)
    SV = dscr("SV", [128 + T, SKV], BF16)
    AR = dscr("AR", [D, T], F32); AS = dscr("AS", [D, T], F32)
    RETT = dscr("RETT", [RV, T], BF16); SWAT = dscr("SWAT", [SQ, T], BF16)
    M1 = dscr("M1", [D, T], F32); MT = dscr("MT", [D, T], BF16)
    X2 = dscr("X2", [T, D], F32); X3 = dscr("X3", [T, D], F32)
    SLOC = dscr("SLOC", [128, H * 256], F32); SGA = dscr("SGA", [8 * 128, H * 256], F32)
    WTD = dscr("WTD", [T // TQ, NE, TQ], F32)
    XNT = dscr("XNT", [D, T], BF16)
    YT = dscr("YT", [D, T], F32)

    dbg_outs = {}

    with ExitStack() as st:
        kb = KB(nc, st)
        st.enter_context(nc.Block())
        for e in ENGS:
            kb.sem("%s0" % e)
        op = kb.op
        uid = [0]

        def sb(stack, name, shape, dt=F32):
            uid[0] += 1
            return stack.enter_context(nc.sbuf_tensor("%s_%d" % (name, uid[0]), list(shape), dt))

        NPSF = int(os.environ.get('KPS', '6'))
        psf = [st.enter_context(nc.psum_tensor("psf%d" % i, [128, 512], F32)) for i in range(NPSF)]
        psb = [st.enter_context(nc.psum_tensor("psb%d" % i, [128, 512], BF16)) for i in range(2)]
        rr = {"f": 0, "b": 0}

        def nps():
            i = rr["f"]; rr["f"] = (i + 1) % NPSF
            return psf[i], "psf%d" % i

        def npb():
            i = rr["b"]; rr["b"] = (i + 1) % 2
            return psb[i], "psb%d" % i

        idb = sb(st, "idb", [128, 128], BF16); idf = sb(st, "idf", [128, 128]); ones = sb(st, "ones", [128, 128], BF16)
        op("sync", lambda e: e.dma_start(out=idb[:], in_=idb_in[:, :]), writes=["idb"], dma="const")
        op("sync", lambda e: e.nop(), reads=["idb"])
        op("sync", lambda e: e.dma_start(out=idf[:], in_=idf_in[:, :]), writes=["idf"], dma="const")
        op("sync", lambda e: e.nop(), reads=["idf"])
        op("sync", lambda e: e.dma_start(out=ones[:], in_=ones_in[:, :]), writes=["ones"], dma="const")
        op("sync", lambda e: e.nop(), reads=["ones"])

        lastcc = []
        SERCC = not os.environ.get('KPARCC')

        def prep(name, src, rows_sh, col_chunks=None, row_chunks=None):
            outs = []
            if NOAG:
                if col_chunks is None:
                    col_chunks = [(0, src.shape[1])]
                if row_chunks is None:
                    row_chunks = [(0, rows_sh)]
                ci = 0
                for (r0, r1) in row_chunks:
                    for (c0, c1) in col_chunks:
                        wgd = din("dbgw_%s_%d" % (name, ci), [8 * (r1 - r0), c1 - c0], BF16)
                        outs.append((wgd, ("W", name, ci)))
                        ci += 1
                return outs
            if col_chunks is None:
                col_chunks = [(0, src.shape[1])]
            if row_chunks is None:
                row_chunks = [(0, rows_sh)]
            ci = 0
            for (r0, r1) in row_chunks:
                for (c0, c1) in col_chunks:
                    nr, ncol = r1 - r0, c1 - c0
                    wb = dscr("wb_%s_%d" % (name, ci), [nr, ncol], BF16)
                    wg = dscr("wg_%s_%d" % (name, ci), [8 * nr, ncol], BF16) if NW > 1 else wb
                    rp = max(1, min(nr, (4 << 20) // (ncol * 4)))
                    keys = []
                    for p0 in range(0, nr, rp):
                        p1 = min(nr, p0 + rp)
                        key = ("wb", name, ci, p0)
                        keys.append(key)
                        op("gpsimd", lambda e, p0=p0, p1=p1, wb=wb: e.dma_start(out=wb[p0:p1, :], in_=src[r0 + p0:r0 + p1, c0:c1]),
                           writes=[key], dma="cast")
                    if NW > 1:
                        op("gpsimd", lambda e, wb=wb, wg=wg: e.collective_compute(
                            "AllGather", ALU.bypass, replica_groups=[list(range(NCORES))], ins=[wb.opt()], outs=[wg.opt()]),
                           reads=keys + lastcc, writes=[("W", name, ci)], dma="cc", dma_inc=1)
                        lastcc[:] = [("W", name, ci)] if SERCC else []
                    else:
                        op("gpsimd", lambda e: e.nop(), reads=keys, writes=[("W", name, ci)])
                    outs.append((wg, ("W", name, ci)))
                    ci += 1
            return outs

        Win = prep("in", w_in_sh, D // NW, col_chunks=[(i, i + c.CW) for i in range(0, c.INW, c.CW)])
        Wro = prep("ro", w_ro_sh, RV // NW)[0]
        Wso = prep("so", w_so_sh, SQ // NW)[0]
        Wmo = prep("mo", w_mo_sh, D // NW)[0]
        Wxkv = prep("xkv", w_xkv_sh, D // NW)[0]
        Wxq = prep("xq", w_xq_sh, D // NW)[0]
        Wxo = prep("xo", w_xo_sh, XW // NW)[0]
        Wr = prep("r", w_r_sh, D // NW)[0]
        Wg = prep("g", w_g_sh, EL * D, row_chunks=[(j * D, (j + 1) * D) for j in range(EL)])
        Wu = prep("u", w_u_sh, EL * D, row_chunks=[(j * D, (j + 1) * D) for j in range(EL)])
        Wd = prep("d", w_d_sh, EL * DE, row_chunks=[(j * DE, (j + 1) * DE) for j in range(EL)])

        class NormT:
            def __init__(self, ph, tag):
                self.tag = tag
                self.xin = [sb(ph, "xin", [128, D]) for _ in range(2)]
                self.junk = sb(ph, "junk", [128, D], BF16)
                self.hb = sb(ph, "hb", [128, D], BF16)
                self.ss = sb(ph, "ss", [128, 1]); self.ms = sb(ph, "ms", [128, 1])
                self.sd = sb(ph, "sd", [128, 1]); self.rs = sb(ph, "rs", [128, 1])
                self.n = 0

            def run(self, src, nchunks, gain, gain_res, dstT, dst_res, tok_off=0):
                tag = self.tag
                junk, hb, ss, ms, sd, rs = self.junk, self.hb, self.ss, self.ms, self.sd, self.rs
                keys = []
                for ci in range(nchunks):
                    b = self.n % 2
                    self.n += 1
                    xin = self.xin[b]
                    xr = "%sxin%d" % (tag, b)
                    op("sync", lambda e: e.dma_start(out=xin[:], in_=src[ci * 128:(ci + 1) * 128, :]), writes=[xr], dma=xr)
                    KN = int(os.environ.get('KN', '9'))
                    if KN < 1:
                        continue
                    op("scalar", lambda e: e.activation(out=junk[:], in_=xin[:], func=AF.Square, accum_out=ss[:]),
                       reads=[xr], writes=[tag + "junk", tag + "ss"])
                    op("vector", lambda e: e.tensor_scalar(out=ms[:], in0=ss[:], scalar1=1.0 / D, scalar2=RMS_EPS, op0=ALU.mult, op1=ALU.add),
                       reads=[tag + "ss"], writes=[tag + "ms"])
                    op("scalar", lambda e: e.activation(out=sd[:], in_=ms[:], func=AF.Sqrt), reads=[tag + "ms"], writes=[tag + "sd"])
                    op("vector", lambda e: e.reciprocal(out=rs[:], in_=sd[:]), reads=[tag + "sd"], writes=[tag + "rs"])
                    if KN < 2:
                        continue
                    op("vector", lambda e: e.scalar_tensor_tensor(out=hb[:], in0=xin[:], scalar=rs[:, 0:1], in1=gain[:], op0=ALU.mult, op1=ALU.mult),
                       reads=[xr, tag + "rs", gain_res], writes=[tag + "hb"])
                    if KN < 3:
                        continue
                    for k4 in range(0, KD, 4):
                        nk = min(4, KD - k4)
                        pt, pr = npb()

                        def tr(e):
                            for j in range(nk):
                                i = e.transpose(pt[:, j * 128:(j + 1) * 128], hb[:, (k4 + j) * 128:(k4 + j + 1) * 128], idb[:])
                            return i
                        op("tensor", tr, reads=[tag + "hb", "idb"], writes=[pr])
                        t0 = tok_off + ci * 128
                        dst = dstT[:, k4:k4 + nk, t0:t0 + 128]
                        srcp = pt[:, 0:nk * 128].rearrange("p (c t) -> p c t", c=nk)
                        key = (dst_res, t0, k4)
                        for j in range(nk):
                            if os.environ.get('KALLDVE'):
                                op("vector", lambda e: e.tensor_copy(out=dstT[:, k4 + j, t0:t0 + 128], in_=pt[:, j * 128:(j + 1) * 128]), reads=[pr], writes=[key + (j,)])
                            else:
                                op("scalar", lambda e: e.activation(out=dstT[:, k4 + j, t0:t0 + 128], in_=pt[:, j * 128:(j + 1) * 128], func=AF.Copy), reads=[pr], writes=[key + (j,)])
                            keys.append(key + (j,))
                return keys

        def load_gain(ph, src, n, name):
            g = sb(ph, name, [128, n])
            op("sync", lambda e: e.dma_start(out=g[:], in_=src[0:1, :].partition_broadcast(128)), writes=[name], dma="const")
            op("sync", lambda e: e.nop(), reads=[name])
            return g

        class WStream:
            def __init__(self, ph, KCmax, WC, nbuf, tag):
                self.t = [sb(ph, "wt" + tag, [128, KCmax, WC], BF16) for _ in range(nbuf)]
                self.i = 0
                self.tag = tag

            def load(self, W, wres, KC, c0, ncols, r0=0):
                b = self.i % len(self.t)
                self.i += 1
                t = self.t[b]
                res = "wt%s%d" % (self.tag, b)
                src = W[r0:r0 + KC * 128, c0:c0 + ncols].rearrange("(c p) n -> p c n", p=128)
                op("sync", lambda e: e.dma_start(out=t[:, 0:KC, 0:ncols], in_=src), reads=[wres], writes=[res], dma=res)
                return t, res

        def mm_group(ps, pres, pairs, reads):
            n = len(pairs)

            def f(e):
                for i, (l, r) in enumerate(pairs):
                    ins = e.matmul(ps, l, r, start=(i == 0), stop=(i == n - 1))
                return ins
            op("tensor", f, reads=reads, writes=[pres])

        for gseg in range(NHS + c.NSG):
            hist = gseg < NHS
            seg = 0 if hist else gseg - NHS
            x_in = xhist[gseg * T:(gseg + 1) * T, :] if hist else x_all[seg * T:(seg + 1) * T, :]
            xh_in = xh_all[seg]
            cs_in = cs_hist[gseg] if hist else cs_all[seg]
            y_out = y_all[seg * T:(seg + 1) * T, :]
            if STOP >= 2:
                o_rq, o_rk, o_rv, o_rg, o_sq, o_sk, o_sv, o_ar, o_as = c.offs[:9]
                with ExitStack() as ph:
                    gmix = load_gain(ph, g_mix, D, "gmix")
                    cs = sb(ph, "cs", [128, NCH, 4, 64])
                    if not os.environ.get('KNOCS'):
                        op("sync", lambda e: e.dma_start(out=cs[:], in_=cs_in[:, :, :, :]), writes=["cs"], dma="const")
                        op("sync", lambda e: e.nop(), reads=["cs"])
                    hT = sb(ph, "hT", [128, KD, TR], BF16)
                    hTh = sb(ph, "hTh", [128, KD, 128], BF16)
                    ws = WStream(ph, KD, 256, 2, "a")
                    nt = NormT(ph, "n1")
                    stg = [sb(ph, "stg", [128, 512]) for _ in range(3)]
                    stb = [sb(ph, "stb", [128, 512], BF16) for _ in range(3)]
                    rt = [sb(ph, "rt", [128, 4, 128]) for _ in range(2)]
                    sctr = {"g": 0, "b": 0, "r": 0}

                    def nstg():
                        i = sctr["g"]; sctr["g"] = (i + 1) % 3
                        return stg[i], "stg%d" % i

                    def nstb():
                        i = sctr["b"]; sctr["b"] = (i + 1) % 3
                        return stb[i], "stb%d" % i

                    KP1 = os.environ.get('KP1', 'z')
                    halo_keys = nt.run(xh_in, 1, gmix, "gmix", hTh, "hTh") if (KP1 >= 'b' and not hist) else []
                    for half in range(T // TR if KP1 >= 'c' else 0):
                        tok0 = half * TR
                        hkeys = nt.run(x_in[tok0:tok0 + TR, :], TR // 128, gmix, "gmix", hT, "hT")
                        for c0 in range(0, c.INW, 256):
                            slot = max(i for i in range(9) if c.offs[i] <= c0)
                            if os.environ.get('KSLOTS') is not None and str(slot) not in os.environ['KSLOTS']:
                                continue
                            if hist and slot not in (1, 2):
                                continue
                            lc = c0 - c.offs[slot]
                            Wc, wres = Win[c0 // c.CW]
                            wt, wtr = ws.load(Wc, wres, KD, c0 % c.CW, 256)
                            if slot in (0, 1, 2, 3, 6):
                                chunks = list(range(TR // 128))
                                if slot == 6 and half == 0:
                                    chunks = [-1] + chunks
                                for tc in chunks:
                                    ps, pr = nps()
                                    if tc < 0:
                                        pairs = [(hTh[:, k, :], wt[:, k, 0:256]) for k in range(KD)]
                                        rd = halo_keys + [wtr]
                                    else:
                                        pairs = [(hT[:, k, tc * 128:(tc + 1) * 128], wt[:, k, 0:256]) for k in range(KD)]
                                        rd = hkeys + [wtr]
                                    mm_group(ps[:, 0:256], pr, pairs, rd)
                                    n_glob = (tok0 // 128) + tc
                                    rows = slice(tok0 + tc * 128, tok0 + (tc + 1) * 128)
                                    if slot in (0, 1):
                                        ob, obr = nstb()
                                        ri = sctr["r"]; sctr["r"] = (ri + 1) % 2
                                        r_t, rres = rt[ri], "rt%d" % ri
                                        pv = ps[:, 0:256].rearrange("p (h t d) -> p h t d", h=2, t=2)
                                        x1, x2 = pv[:, :, 0, :], pv[:, :, 1, :]
                                        co = cs[:, n_glob, 2 * slot, :].unsqueeze(1).to_broadcast([128, 2, 64])
                                        si = cs[:, n_glob, 2 * slot + 1, :].unsqueeze(1).to_broadcast([128, 2, 64])
                                        rv4 = r_t[:].rearrange("p a (h d) -> p a h d", h=2)
                                        for j, (a, bb) in enumerate([(x1, co), (x2, si), (x1, si), (x2, co)]):
                                            op("vector", lambda e, j=j, a=a, bb=bb: e.tensor_tensor(out=rv4[:, j], in0=a, in1=bb, op=ALU.mult),
                                               reads=[pr, "cs"], writes=[(rres, j)])
                                        ov = ob[:, 0:256].rearrange("p (h t d) -> p h t d", h=2, t=2)
                                        op("gpsimd", lambda e: e.tensor_tensor(out=ov[:, :, 0, :], in0=rv4[:, 0], in1=rv4[:, 1], op=ALU.subtract),
                                           reads=[(rres, 0), (rres, 1)], writes=[(obr, 0)])
                                        op("gpsimd", lambda e: e.tensor_tensor(out=ov[:, :, 1, :], in0=rv4[:, 2], in1=rv4[:, 3], op=ALU.add),
                                           reads=[(rres, 2), (rres, 3)], writes=[(obr, 1)])
                                        dst = (RQ if slot == 0 else RK)[rows, lc:lc + 256]
                                        op("scalar", lambda e: e.dma_start(out=dst, in_=ob[:, 0:256]), reads=[(obr, 0), (obr, 1)],
                                           writes=[("RQ" if slot == 0 else "RK", n_glob, lc), obr], dma=obr)
                                    elif slot == 2:
                                        ob, obr = nstb()
                                        op("scalar", lambda e: e.activation(out=ob[:, 0:256], in_=ps[:, 0:256], func=AF.Copy), reads=[pr], writes=[obr])
                                        op("scalar", lambda e: e.dma_start(out=RVs[rows, lc:lc + 256], in_=ob[:, 0:256]), reads=[obr],
                                           writes=[("RV", n_glob, lc)], dma=obr)
                                    elif slot == 3:
                                        og, ogr = nstg()
                                        op("scalar", lambda e: e.activation(out=og[:, 0:256], in_=ps[:, 0:256], func=AF.Silu), reads=[pr], writes=[ogr])
                                        op("scalar", lambda e: e.dma_start(out=RG[rows, lc:lc + 256], in_=og[:, 0:256]), reads=[ogr],
                                           writes=[("RG", n_glob, lc)], dma=ogr)
                                    else:
                                        ob, obr = nstb()
                                        op("vector", lambda e: e.tensor_copy(out=ob[:, 0:256], in_=ps[:, 0:256]), reads=[pr], writes=[obr])
                                        r2 = slice(128 + tok0 + tc * 128, 128 + tok0 + (tc + 1) * 128) if tc >= 0 else slice(0, 128)
                                        op("scalar", lambda e: e.dma_start(out=SV[r2, lc:lc + 256], in_=ob[:, 0:256]), reads=[obr],
                                           writes=[("SV", n_glob, lc)], dma=obr)
                            else:
                                M = 64 if slot in (4, 5) else 128
                                for m in range(256 // M):
                                    tiles = [(tt, TT) for tt in range(TR // TT)]
                                    if slot == 5 and half == 0:
                                        tiles = [(-1, 128)] + tiles
                                    for (tt, ntk) in tiles:
                                        ps, pr = nps()
                                        if tt < 0:
                                            pairs = [(wt[:, k, m * M:(m + 1) * M], hTh[:, k, :]) for k in range(KD)]
                                            rd = halo_keys + [wtr]
                                        else:
                                            pairs = [(wt[:, k, m * M:(m + 1) * M], hT[:, k, tt * TT:(tt + 1) * TT]) for k in range(KD)]
                                            rd = hkeys + [wtr]
                                        mm_group(ps[0:M, 0:ntk], pr, pairs, rd)
                                        fi = (lc + m * M) // M
                                        tsl = slice(tok0 + tt * TT, tok0 + (tt + 1) * TT)
                                        if slot == 4:
                                            ob, obr = nstb()
                                            op("scalar", lambda e: e.activation(out=ob[0:64, 0:ntk], in_=ps[0:64, 0:ntk], func=AF.Copy),
                                               reads=[pr], writes=[obr])
                                            op("scalar", lambda e: e.dma_start(out=SQT[fi, :, tsl], in_=ob[0:64, 0:ntk]), reads=[obr],
                                               writes=[("SQT", fi, half, tt)], dma=obr)
                                        elif slot == 5:
                                            ob, obr = nstb()
                                            op("vector", lambda e: e.tensor_copy(out=ob[0:64, 0:ntk], in_=ps[0:64, 0:ntk]), reads=[pr], writes=[obr])
                                            ks = slice(128 + tok0 + tt * TT, 128 + tok0 + (tt + 1) * TT) if tt >= 0 else slice(0, 128)
                                            op("scalar", lambda e: e.dma_start(out=SKT[fi, :, ks], in_=ob[0:64, 0:ntk]), reads=[obr],
                                               writes=[("SKT", fi, half, tt)], dma=obr)
                                        else:
                                            og, ogr = nstg()
                                            op("scalar", lambda e: e.activation(out=og[:, 0:ntk], in_=ps[:, 0:ntk], func=AF.Sigmoid), reads=[pr], writes=[ogr])
                                            dstt = (AR if slot == 7 else AS)[fi * 128:(fi + 1) * 128, tsl]
                                            op("scalar", lambda e: e.dma_start(out=dstt, in_=og[:, 0:ntk]), reads=[ogr],
                                               writes=[("AR" if slot == 7 else "AS", fi, half, tt)], dma=ogr)
                kb.barrier()

            if STOP >= 3:
                with ExitStack() as ph:
                    dec = sb(ph, "dec", [128, H, 128])
                    qkd = sb(ph, "qkd", [128, 3, H])
                    coef = sb(ph, "coef", [128, 8, H])
                    gret = load_gain(ph, g_ret, RV, "gret")
                    op("sync", lambda e: e.dma_start(out=dec[:], in_=dec_in[:, :, :]), writes=["dec"], dma="const")
                    op("sync", lambda e: e.nop(), reads=["dec"])
                    op("sync", lambda e: e.dma_start(out=qkd[:], in_=qkd_in[:, :, :]), writes=["qkd"], dma="const")
                    op("sync", lambda e: e.nop(), reads=["qkd"])
                    op("sync", lambda e: e.dma_start(out=coef[:], in_=coef_in[:, :, :]), writes=["coef"], dma="const")
                    op("sync", lambda e: e.nop(), reads=["coef"])
                    R = [sb(ph, "R", [128, H, 256]) for _ in range(2)]
                    Rb = sb(ph, "Rb", [128, H, 256], BF16)
                    qt = [sb(ph, "qt", [128, QK], BF16) for _ in range(2)]
                    kt = [sb(ph, "kt", [128, QK], BF16) for _ in range(2)]
                    vt = [sb(ph, "vt", [128, RV], BF16) for _ in range(2)]
                    gt = [sb(ph, "gt", [128, RV]) for _ in range(2)]
                    kdc = [sb(ph, "kdc", [128, H, 128], BF16) for _ in range(2)]
                    qkT = [sb(ph, "qkT", [128, 256], BF16) for _ in range(2)]
                    PT = [sb(ph, "PT", [128, 128], BF16) for _ in range(2)]
                    Bs = [sb(ph, "Bs", [128, 256]) for _ in range(2)]
                    oc = sb(ph, "oc", [128, H, 256])
                    yb = sb(ph, "yb", [128, RV], BF16)
                    ssr = sb(ph, "ssr", [128, H]); msr = sb(ph, "msr", [128, H]); sdr = sb(ph, "sdr", [128, H]); rsr = sb(ph, "rsr", [128, H])
                    jr = sb(ph, "jr", [128, 256], BF16)
                    retS = [sb(ph, "retS", [128, RV // 128, 256], BF16) for _ in range(2)]
                    sg_t = gt

                    def load_kv(n, need_q):
                        b = n % 2
                        rows = slice(n * 128, (n + 1) * 128)
                        rd_k = [("RK", n, l) for l in range(0, QK, 256)]
                        rd_v = [("RV", n, l) for l in range(0, RV, 256)]
                        op("sync", lambda e: e.dma_start(out=kt[b][:], in_=RK[rows, :]), reads=rd_k, writes=["kt%d" % b], dma="kt%d" % b)
                        op("sync", lambda e: e.dma_start(out=vt[b][:], in_=RVs[rows, :]), reads=rd_v, writes=["vt%d" % b], dma="vt%d" % b)
                        if need_q:
                            rd_q = [("RQ", n, l) for l in range(0, QK, 256)]
                            rd_g = [("RG", n, l) for l in range(0, RV, 256)]
                            op("sync", lambda e: e.dma_start(out=qt[b][:], in_=RQ[rows, :]), reads=rd_q, writes=["qt%d" % b], dma="qt%d" % b)
                            op("sync", lambda e: e.dma_start(out=gt[b][:], in_=RG[rows, :]), reads=rd_g, writes=["gt%d" % b], dma="gt%d" % b)
                        op("gpsimd", lambda e: e.tensor_tensor(out=kdc[b][:], in0=kt[b][:].rearrange("p (h d) -> p h d", h=H),
                                                               in1=qkd[:, 1, :].unsqueeze(2).to_broadcast([128, H, 128]), op=ALU.mult),
                           reads=["kt%d" % b, "qkd"], writes=["kdc%d" % b])
                        return b

                    def state_update(b, cur, h):
                        ps, pr = nps()
                        mm_group(ps[:, 0:256], pr, [(kdc[b][:, h, :], vt[b][:, h * 256:(h + 1) * 256])], ["kdc%d" % b, "vt%d" % b])
                        op("vector", lambda e: e.scalar_tensor_tensor(out=R[1 - cur][:, h, :], in0=R[cur][:, h, :], scalar=qkd[:, 2, h:h + 1],
                                                                      in1=ps[:, 0:256], op0=ALU.mult, op1=ALU.add),
                           reads=[pr, ("R", cur, h), "qkd"], writes=[("R", 1 - cur, h)])

                    if NW == 1 and gseg > 0:
                        op("sync", lambda e: e.dma_start(out=R[0][:].rearrange("p h d -> p (h d)"), in_=RST[:, :]), writes=[("R", 0, h) for h in range(H)], dma="rst")
                    else:
                        for h in range(H):
                            op("gpsimd", lambda e, h=h: e.memset(R[0][:, h, :], 0.0), writes=[("R", 0, h)])
                    cur = 0
                    for n in range(NCH if ((NW > 1 and NS > 1) or hist) else 0):
                        b = load_kv(n, False)
                        for h in range(H):
                            state_update(b, cur, h)
                        cur = 1 - cur
                    if NW > 1 and NS > 1:
                        op("sync", lambda e: e.dma_start(out=SLOC[:, :], in_=R[cur][:].rearrange("p h d -> p (h d)")),
                           reads=[("R", cur, h) for h in range(H)], writes=["SLOC"], dma="sloc")
                        if not NOAG:
                            op("gpsimd", lambda e: e.collective_compute("AllGather", ALU.bypass, replica_groups=[list(range(NCORES))],
                                                                        ins=[SLOC.opt()], outs=[SGA.opt()]),
                               reads=["SLOC"], writes=["SGA"], dma="cc", dma_inc=1)
                        else:
                            for j in range(8):
                                op("sync", lambda e: e.dma_start(out=SGA[j * 128:(j + 1) * 128, :], in_=SLOC[:, :]), reads=["SLOC"], writes=[("SGA", j)], dma="sgad")
                        cur2 = 1 - cur
                        for h in range(H):
                            op("gpsimd", lambda e, h=h: e.memset(R[cur2][:, h, :], 0.0), writes=[("R", cur2, h)])
                        for j in range(8):
                            sgb = sg_t[j % 2]
                            sgr = "gt%d" % (j % 2)
                            op("sync", lambda e: e.dma_start(out=sgb[:], in_=SGA[j * 128:(j + 1) * 128, :]), reads=["SGA", ("SGA", j)], writes=[sgr], dma=sgr)
                            for h in range(H):
                                op("vector", lambda e, h=h: e.scalar_tensor_tensor(out=R[1 - cur2][:, h, :], in0=sgb[:, h * 256:(h + 1) * 256],
                                                                                   scalar=coef[:, j, h:h + 1], in1=R[cur2][:, h, :],
                                                                                   op0=ALU.mult, op1=ALU.add),
                                   reads=[sgr, "coef", ("R", cur2, h)], writes=[("R", 1 - cur2, h)])
                            cur2 = 1 - cur2
                        cur = cur2
                    for h in range(H if not hist else 0):
                        op("scalar", lambda e, h=h: e.activation(out=Rb[:, h, :], in_=R[cur][:, h, :], func=AF.Copy),
                           reads=[("R", cur, h)], writes=[("Rb", h)])
                    for n in range(NCH if not hist else 0):
                        b = load_kv(n, True)
                        for h in range(H):
                            i2 = h % 2
                            pt, ptr = npb()

                            def tr(e, pt=pt, h=h):
                                e.transpose(pt[:, 0:128], qt[b][:, h * 128:(h + 1) * 128], idb[:])
                                return e.transpose(pt[:, 128:256], kt[b][:, h * 128:(h + 1) * 128], idb[:])
                            op("tensor", tr, reads=["qt%d" % b, "kt%d" % b, "idb"], writes=[ptr])
                            op("scalar", lambda e, pt=pt: e.activation(out=qkT[i2][:], in_=pt[:, 0:256], func=AF.Copy), reads=[ptr], writes=["qkT%d" % i2])
                            psA, prA = nps()
                            mm_group(psA[:, 0:128], prA, [(qkT[i2][:, 128:256], qkT[i2][:, 0:128])], ["qkT%d" % i2])
                            op("vector", lambda e, psA=psA, h=h: e.tensor_tensor(out=PT[i2][:], in0=psA[:, 0:128], in1=dec[:, h, :], op=ALU.mult),
                               reads=[prA, "dec"], writes=["PT%d" % i2])
                            psB, prB = nps()
                            mm_group(psB[:, 0:256], prB, [(PT[i2][:], vt[b][:, h * 256:(h + 1) * 256])], ["PT%d" % i2, "vt%d" % b])
                            psC, prC = nps()
                            mm_group(psC[:, 0:256], prC, [(qkT[i2][:, 0:128], Rb[:, h, :])], ["qkT%d" % i2, ("Rb", h)])
                            op("scalar", lambda e, psB=psB: e.activation(out=Bs[i2][:], in_=psB[:, 0:256], func=AF.Copy), reads=[prB], writes=["Bs%d" % i2])
                            op("vector", lambda e, psC=psC, h=h: e.scalar_tensor_tensor(out=oc[:, h, :], in0=psC[:, 0:256], scalar=qkd[:, 0, h:h + 1],
                                                                                        in1=Bs[i2][:], op0=ALU.mult, op1=ALU.add),
                               reads=[prC, "Bs%d" % i2, "qkd"], writes=[("oc", h)])
                            state_update(b, cur, h)
                            op("scalar", lambda e, h=h: e.activation(out=Rb[:, h, :], in_=R[1 - cur][:, h, :], func=AF.Copy),
                               reads=[("R", 1 - cur, h)], writes=[("Rb", h)])
                            op("scalar", lambda e, h=h: e.activation(out=jr[:], in_=oc[:, h, :], func=AF.Square, accum_out=ssr[:, h:h + 1]),
                               reads=[("oc", h)], writes=["jr", ("ssr", h)])
                        cur = 1 - cur
                        op("vector", lambda e: e.tensor_scalar(out=msr[:], in0=ssr[:], scalar1=1.0 / 256, scalar2=RMS_EPS, op0=ALU.mult, op1=ALU.add),
                           reads=[("ssr", h) for h in range(H)], writes=["msr"])
                        op("scalar", lambda e: e.activation(out=sdr[:], in_=msr[:], func=AF.Sqrt), reads=["msr"], writes=["sdr"])
                        op("vector", lambda e: e.reciprocal(out=rsr[:], in_=sdr[:]), reads=["sdr"], writes=["rsr"])
                        for h in range(H):
                            op("vector", lambda e, h=h: e.scalar_tensor_tensor(out=oc[:, h, :], in0=oc[:, h, :], scalar=rsr[:, h:h + 1],
                                                                               in1=gret[:, h * 256:(h + 1) * 256], op0=ALU.mult, op1=ALU.mult),
                               reads=[("oc", h), "rsr", "gret"], writes=[("oc", h)])
                        op("gpsimd", lambda e: e.tensor_tensor(out=yb[:], in0=oc[:].rearrange("p h d -> p (h d)"), in1=gt[b][:], op=ALU.mult),
                           reads=[("oc", h) for h in range(H)] + ["gt%d" % b], writes=["yb"])
                        grp = n // 2
                        rS = retS[grp % 2]
                        rSr = "retS%d" % (grp % 2)
                        for k4 in range(0, RV // 128, 4):
                            pt, ptr = npb()

                            def tr2(e, pt=pt, k4=k4):
                                for j in range(4):
                                    i = e.transpose(pt[:, j * 128:(j + 1) * 128], yb[:, (k4 + j) * 128:(k4 + j + 1) * 128], idb[:])
                                return i
                            op("tensor", tr2, reads=["yb", "idb"], writes=[ptr])
                            for j in range(4):
                                dst = rS[:, k4 + j, (n % 2) * 128:(n % 2 + 1) * 128]
                                if False:
                                    pass
                                else:
                                    op("scalar", lambda e: e.activation(out=dst, in_=pt[:, j * 128:(j + 1) * 128], func=AF.Copy), reads=[ptr], writes=[(rSr, n % 2, k4, j)])
                        if n % 2 == 1 or n == NCH - 1:
                            ng = (n % 2) + 1
                            t0 = grp * 256
                            op("scalar", lambda e, rS=rS, t0=t0, ng=ng: e.dma_start(
                                out=RETT[:, t0:t0 + ng * 128].rearrange("(c p) t -> p c t", p=128), in_=rS[:, :, 0:ng * 128]),
                               reads=[(rSr, q, k4, j) for q in range(ng) for k4 in range(0, RV // 128, 4) for j in range(4)],
                               writes=[("RETT", grp)] + [(rSr, q, k4, j) for q in range(ng) for k4 in range(0, RV // 128, 4) for j in range(4)], dma=rSr)
                    if NW == 1 and gseg < NHS + NSG - 1:
                        op("sync", lambda e: e.dma_start(out=RST[:, :], in_=R[cur][:].rearrange("p h d -> p (h d)")),
                           reads=[("R", cur, h) for h in range(H)], writes=["RST"], dma="rst")
                kb.barrier()

            if STOP >= 4 and not hist:
                HB = min(4, G)
                NHB = G // HB
                with ExitStack() as ph:
                    msk = sb(ph, "msk", [128, 3, 512], BF16)
                    op("sync", lambda e: e.dma_start(out=msk[:], in_=msk_in[:, :, :]), writes=["msk"], dma="const")
                    op("sync", lambda e: e.nop(), reads=["msk"])
                    snk = sb(ph, "snk", [128, HQ]); esk = sb(ph, "esk", [128, HQ])
                    op("sync", lambda e: e.dma_start(out=snk[:], in_=sinks_in[0:1, :].partition_broadcast(128)), writes=["snk"], dma="const")
                    op("sync", lambda e: e.nop(), reads=["snk"])
                    op("scalar", lambda e: e.activation(out=esk[:], in_=snk[:], func=AF.Exp), reads=["snk"], writes=["esk"])
                    sq_t = [sb(ph, "sq", [64, G, T], BF16) for _ in range(2)]
                    sk_t = [sb(ph, "sk", [64, 128 + T], BF16) for _ in range(2)]
                    sv_t = [sb(ph, "sv", [128, NCH + 1, 65], BF16) for _ in range(2)]
                    for b in range(2):
                        op("gpsimd", lambda e, b=b: e.memset(sv_t[b][:, :, 64:65], 1.0), writes=["sv%d" % b])
                    Pe = [sb(ph, "Pe", [128, 512], BF16) for _ in range(4)]
                    Pm = [sb(ph, "Pm", [128, 512], BF16) for _ in range(4)]
                    den = [sb(ph, "den", [128, 4]) for _ in range(2)]
                    rec = [sb(ph, "rec", [128, 4]) for _ in range(2)]
                    otk = [sb(ph, "otk", [128, G * 64], BF16) for _ in range(2)]
                    swS = [sb(ph, "swS", [128, max(1, G * 64 // 128), 512], BF16) for _ in range(2)]
                    pctr = [0]
                    dctr = [0]
                    NB = max(1, G * 64 // 128)
                    for g in range(HKV):
                        b = g % 2
                        op("sync", lambda e: e.dma_start(out=sq_t[b][:], in_=SQT[g * G:(g + 1) * G, :, :].rearrange("h d t -> d h t")),
                           reads=[("SQT", g * G + hh, half, tt) for hh in range(G) for half in range(T // TR) for tt in range(TR // TT)],
                           writes=["sq%d" % b], dma="sq%d" % b)
                        op("sync", lambda e: e.dma_start(out=sk_t[b][:], in_=SKT[g, :, :]),
                           reads=[("SKT", g, half, tt) for half in range(T // TR) for tt in range(TR // TT)] + [("SKT", g, 0, -1)],
                           writes=["sk%d" % b], dma="sk%d" % b)
                        lcb = (g * 64) // 256 * 256
                        op("sync", lambda e: e.dma_start(out=sv_t[b][:, :, 0:64], in_=SV[:, g * 64:(g + 1) * 64].rearrange("(c p) d -> p c d", p=128)),
                           reads=[("SV", n, lcb) for n in range(-1, NCH)], writes=["sv%d" % b], dma="sv%d" % b)
                        for n in range(NCH):
                            ob = otk[n % 2]
                            obr = "otk%d" % (n % 2)
                            for hb in range(NHB):
                                pms = []
                                for kt_ in range(2):
                                    ps, pr = nps()
                                    mm_group(ps[:, 0:HB * 128].rearrange("p (h t) -> p h t", h=HB), pr,
                                             [(sk_t[b][:, (n + kt_) * 128:(n + kt_ + 1) * 128], sq_t[b][:, hb * HB:(hb + 1) * HB, n * 128:(n + 1) * 128])],
                                             ["sk%d" % b, "sq%d" % b])
                                    pi = pctr[0] % 4
                                    pctr[0] += 1
                                    op("scalar", lambda e, ps=ps, pi=pi: e.activation(out=Pe[pi][:, 0:HB * 128], in_=ps[:, 0:HB * 128], func=AF.Exp, scale=0.125),
                                       reads=[pr], writes=["Pe%d" % pi])
                                    mi = (2 if (n == 0 and seg == 0) else 0) if kt_ == 0 else 1
                                    op("gpsimd", lambda e, pi=pi, mi=mi: e.tensor_tensor(out=Pm[pi][:, 0:HB * 128], in0=Pe[pi][:, 0:HB * 128],
                                                                                       in1=msk[:, mi, 0:HB * 128], op=ALU.mult),
                                       reads=["Pe%d" % pi, "msk"], writes=["Pm%d" % pi])
                                    pms.append(pi)
                                pso, pro = nps()

                                def pvmm(e, pso=pso, pms=pms):
                                    for hl in range(HB):
                                        for kt_ in range(2):
                                            i = e.matmul(pso[:, hl * 65:(hl + 1) * 65], Pm[pms[kt_]][:, hl * 128:(hl + 1) * 128], sv_t[b][:, n + kt_, :],
                                                         start=(kt_ == 0), stop=(kt_ == 1))
                                    return i
                                op("tensor", pvmm, reads=["Pm%d" % pms[0], "Pm%d" % pms[1], "sv%d" % b], writes=[pro])
                                di = dctr[0] % 2
                                dctr[0] += 1
                                pv = pso[:, 0:HB * 65].rearrange("p (h d) -> p h d", h=HB)
                                hq0 = g * G + hb * HB
                                prs = [pro]
                                op("vector", lambda e, pv=pv, di=di, hq0=hq0: e.tensor_tensor(out=den[di][:, 0:HB], in0=pv[:, :, 64], in1=esk[:, hq0:hq0 + HB], op=ALU.add),
                                   reads=prs + ["esk"], writes=["den%d" % di])
                                op("vector", lambda e, di=di: e.reciprocal(out=rec[di][:, 0:HB], in_=den[di][:, 0:HB]), reads=["den%d" % di], writes=["rec%d" % di])
                                col = hb * HB * 64
                                op("vector", lambda e, pv=pv, di=di, col=col: e.tensor_tensor(
                                    out=ob[:, col:col + HB * 64].rearrange("p (h d) -> p h d", h=HB), in0=pv[:, :, 0:64],
                                    in1=rec[di][:, 0:HB].unsqueeze(2).to_broadcast([128, HB, 64]), op=ALU.mult),
                                   reads=prs + ["rec%d" % di], writes=[(obr, hb)])
                            grp = n // 4
                            sS = swS[(g * ((NCH + 3) // 4) + grp) % 2]
                            sSr = "swS%d" % ((g * ((NCH + 3) // 4) + grp) % 2)
                            pt, ptr = npb()

                            def tr3(e, pt=pt, ob=ob):
                                for j in range(NB):
                                    i = e.transpose(pt[0:min(128, G * 64), j * 128:(j + 1) * 128], ob[:, j * 128:j * 128 + min(128, G * 64)], idb[:])
                                return i
                            op("tensor", tr3, reads=[(obr, hb) for hb in range(NHB)] + ["idb"], writes=[ptr])
                            PW = min(128, G * 64)
                            for j in range(NB):
                                dst = sS[0:PW, j, (n % 4) * 128:(n % 4 + 1) * 128]
                                if False:
                                    pass
                                else:
                                    op("scalar", lambda e: e.activation(out=dst, in_=pt[0:PW, j * 128:(j + 1) * 128], func=AF.Copy), reads=[ptr], writes=[(sSr, n % 4, j)])
                            if n % 4 == 3 or n == NCH - 1:
                                ng = (n % 4) + 1
                                t0 = grp * 512
                                op("scalar", lambda e, sS=sS, t0=t0, ng=ng, g=g: e.dma_start(
                                    out=SWAT[g * G * 64:(g + 1) * G * 64, t0:t0 + ng * 128].rearrange("(c p) t -> p c t", p=PW),
                                    in_=sS[0:PW, 0:NB, 0:ng * 128]),
                                   reads=[(sSr, q, j) for q in range(ng) for j in range(NB)], writes=[("SWAT", g, grp)] + [(sSr, q, j) for q in range(ng) for j in range(NB)], dma=sSr)
                kb.barrier()

            if STOP >= 5 and not hist:
                def load_actT(ph_t, res_name, src, KC, tok0, ntok, src_res):
                    op("sync", lambda e: e.dma_start(out=ph_t[:, 0:KC, 0:ntok], in_=src[:, tok0:tok0 + ntok].rearrange("(c p) t -> p c t", p=128)),
                       reads=src_res, writes=[res_name], dma=res_name)

                with ExitStack() as ph:
                    KCm = max(RV, SQ, D) // 128
                    aT = sb(ph, "aT", [128, KCm, TR], BF16)
                    ws = WStream(ph, KCm, 512, 2, "m")
                    ld = [sb(ph, "ld", [128, 512]) for _ in range(4)]
                    st1 = [sb(ph, "st1", [128, 512]) for _ in range(3)]
                    stb1 = [sb(ph, "stb1", [128, 512], BF16) for _ in range(2)]
                    lctr = [0]

                    def nld():
                        i = lctr[0] % 4
                        lctr[0] += 1
                        return ld[i], "ld%d" % i
                    sc = [0, 0]
                    WCm = min(512, D)
                    for half in range(T // TR):
                        tok0 = half * TR
                        load_actT(aT, "aT", RETT, RV // 128, tok0, TR, [("RETT", gi) for gi in range((NCH + 3) // 4)])
                        for c0 in range(0, D, WCm):
                            wt, wtr = ws.load(Wro[0], Wro[1], RV // 128, c0, WCm)
                            for m in range(WCm // 128):
                                fi = (c0 // 128) + m
                                for tt in range(TR // TT):
                                    tsl = slice(tok0 + tt * TT, tok0 + (tt + 1) * TT)
                                    ps, pr = nps()
                                    mm_group(ps[:, 0:TT], pr, [(wt[:, k, m * 128:(m + 1) * 128], aT[:, k, tt * TT:(tt + 1) * TT]) for k in range(RV // 128)], ["aT", wtr])
                                    la, lar = nld()
                                    op("sync", lambda e, la=la, fi=fi, tsl=tsl: e.dma_start(out=la[:, 0:TT], in_=AR[fi * 128:(fi + 1) * 128, tsl]),
                                       reads=[("AR", fi, h2, t2) for h2 in range(T // TR) for t2 in range(TR // TT)], writes=[lar], dma=lar)
                                    i = sc[0] % 3
                                    sc[0] += 1
                                    so, sor = st1[i], "st1%d" % i
                                    op("vector", lambda e, ps=ps, la=la, so=so: e.tensor_tensor(out=so[:, 0:TT], in0=ps[:, 0:TT], in1=la[:, 0:TT], op=ALU.mult),
                                       reads=[pr, lar], writes=[sor])
                                    op("scalar", lambda e, so=so, fi=fi, tsl=tsl: e.dma_start(out=M1[fi * 128:(fi + 1) * 128, tsl], in_=so[:, 0:TT]),
                                       reads=[sor], writes=[("M1", fi, half, tt)], dma=sor)
                        load_actT(aT, "aT", SWAT, SQ // 128, tok0, TR, [("SWAT", g, gi) for g in range(HKV) for gi in range((NCH + 3) // 4)])
                        for c0 in range(0, D, WCm):
                            wt, wtr = ws.load(Wso[0], Wso[1], SQ // 128, c0, WCm)
                            for m in range(WCm // 128):
                                fi = (c0 // 128) + m
                                for tt in range(TR // TT):
                                    tsl = slice(tok0 + tt * TT, tok0 + (tt + 1) * TT)
                                    ps, pr = nps()
                                    mm_group(ps[:, 0:TT], pr, [(wt[:, k, m * 128:(m + 1) * 128], aT[:, k, tt * TT:(tt + 1) * TT]) for k in range(SQ // 128)], ["aT", wtr])
                                    la, lar = nld()
                                    op("sync", lambda e, la=la, fi=fi, tsl=tsl: e.dma_start(out=la[:, 0:TT], in_=AS[fi * 128:(fi + 1) * 128, tsl]),
                                       reads=[("AS", fi, h2, t2) for h2 in range(T // TR) for t2 in range(TR // TT)], writes=[lar], dma=lar)
                                    lm, lmr = nld()
                                    op("sync", lambda e, lm=lm, fi=fi, tsl=tsl: e.dma_start(out=lm[:, 0:TT], in_=M1[fi * 128:(fi + 1) * 128, tsl]),
                                       reads=[("M1", fi, half, tt)], writes=[lmr], dma=lmr)
                                    i = sc[0] % 3
                                    sc[0] += 1
                                    so, sor = st1[i], "st1%d" % i
                                    op("vector", lambda e, ps=ps, la=la, so=so: e.tensor_tensor(out=so[:, 0:TT], in0=ps[:, 0:TT], in1=la[:, 0:TT], op=ALU.mult),
                                       reads=[pr, lar], writes=[sor])
                                    j = sc[1] % 2
                                    sc[1] += 1
                                    sbo, sbr = stb1[j], "stb1%d" % j
                                    op("gpsimd", lambda e, so=so, lm=lm, sbo=sbo: e.tensor_tensor(out=sbo[:, 0:TT], in0=so[:, 0:TT], in1=lm[:, 0:TT], op=ALU.add),
                                       reads=[sor, lmr], writes=[sbr])
                                    op("scalar", lambda e, sbo=sbo, fi=fi, tsl=tsl: e.dma_start(out=MT[fi * 128:(fi + 1) * 128, tsl], in_=sbo[:, 0:TT]),
                                       reads=[sbr], writes=[("MT", fi, half, tt)], dma=sbr)
                        load_actT(aT, "aT", MT, KD, tok0, TR, [("MT", fi, half, tt) for fi in range(KD) for tt in range(TR // TT)])
                        for c0 in range(0, D, WCm):
                            wt, wtr = ws.load(Wmo[0], Wmo[1], KD, c0, WCm)
                            for tc in range(TR // 128):
                                rows = slice(tok0 + tc * 128, tok0 + (tc + 1) * 128)
                                ps, pr = nps()
                                mm_group(ps[:, 0:WCm], pr, [(aT[:, k, tc * 128:(tc + 1) * 128], wt[:, k, 0:WCm]) for k in range(KD)], ["aT", wtr])
                                la, lar = nld()
                                op("sync", lambda e, la=la, rows=rows, c0=c0: e.dma_start(out=la[:, 0:WCm], in_=x_in[rows, c0:c0 + WCm]), writes=[lar], dma=lar)
                                i = sc[0] % 3
                                sc[0] += 1
                                so, sor = st1[i], "st1%d" % i
                                op("vector", lambda e, ps=ps, la=la, so=so: e.tensor_tensor(out=so[:, 0:WCm], in0=ps[:, 0:WCm], in1=la[:, 0:WCm], op=ALU.add),
                                   reads=[pr, lar], writes=[sor])
                                op("scalar", lambda e, so=so, rows=rows, c0=c0: e.dma_start(out=X2[rows, c0:c0 + WCm], in_=so[:, 0:WCm]),
                                   reads=[sor], writes=[("X2", tok0 // 128 + tc, c0)], dma=sor)
                kb.barrier()

            if STOP >= 6 and not hist:
                XC = min(512, XW)
                TRx = min(T, 512)
                WCm = min(512, D)
                xsc = 1.0 / math.sqrt(256.0)
                with ExitStack() as ph6:
                    kxT = sb(ph6, "kxT", [128, XK, ML], BF16)
                    vx = sb(ph6, "vx", [128, ML // 128, XW], BF16)
                    with ExitStack() as ph:
                        gmem = load_gain(ph, g_mem, D, "gmem")
                        memT = sb(ph, "memT", [128, KD, ML], BF16)
                        ws = WStream(ph, KD, XC, 2, "xa")
                        nt = NormT(ph, "nm")
                        mkeys = nt.run(mem_in, ML // 128, gmem, "gmem", memT, "memT")
                        for c0 in range(0, 2 * XW, XC):
                            wt, wtr = ws.load(Wxkv[0], Wxkv[1], KD, c0, XC)
                            if c0 < XW:
                                for m in range(XC // 128):
                                    fi = c0 // 128 + m
                                    ps, pr = nps()
                                    mm_group(ps[:, 0:ML], pr, [(wt[:, k, m * 128:(m + 1) * 128], memT[:, k, :]) for k in range(KD)], mkeys + [wtr])
                                    op("vector", lambda e: e.tensor_copy(out=kxT[:, fi, :], in_=ps[:, 0:ML]), reads=[pr], writes=[("kxT", fi)])
                            else:
                                for mc in range(ML // 128):
                                    ps, pr = nps()
                                    mm_group(ps[:, 0:XC], pr, [(memT[:, k, mc * 128:(mc + 1) * 128], wt[:, k, 0:XC]) for k in range(KD)], mkeys + [wtr])
                                    cc = c0 - XW
                                    op("vector", lambda e: e.tensor_copy(out=vx[:, mc, cc:cc + XC], in_=ps[:, 0:XC]), reads=[pr], writes=[("vx", mc, cc)])
                    kb.barrier()
                    with ExitStack() as ph:
                        gxat = load_gain(ph, g_xat, D, "gxat")
                        hxT = sb(ph, "hxT", [128, KD, TRx], BF16)
                        qxT = sb(ph, "qxT", [128, XK, TRx], BF16)
                        oxT = sb(ph, "oxT", [128, XK, TRx], BF16)
                        wsq = WStream(ph, KD, 256, 2, "xq")
                        wso = WStream(ph, XK, WCm, 2, "xo")
                        Px = [sb(ph, "Px", [128, TT], BF16) for _ in range(4)]
                        rcx = [sb(ph, "rcx", [128, TT]) for _ in range(2)]
                        ld = [sb(ph, "ldx", [128, 512]) for _ in range(2)]
                        st1 = [sb(ph, "stx", [128, 512]) for _ in range(2)]
                        nt = NormT(ph, "nx")
                        pxc = [0]
                        XQC = min(256, XW)
                        for part in range(T // TRx):
                            tok0 = part * TRx
                            hkeys = nt.run(X2[tok0:tok0 + TRx, :], TRx // 128, gxat, "gxat", hxT, "hxT")
                            for c0 in range(0, XW, XQC):
                                wt, wtr = wsq.load(Wxq[0], Wxq[1], KD, c0, XQC)
                                for m in range(XQC // 128):
                                    fi = c0 // 128 + m
                                    for tt in range(TRx // TT):
                                        ps, pr = nps()
                                        mm_group(ps[:, 0:TT], pr, [(wt[:, k, m * 128:(m + 1) * 128], hxT[:, k, tt * TT:(tt + 1) * TT]) for k in range(KD)], hkeys + [wtr])
                                        op("scalar", lambda e: e.activation(out=qxT[:, fi, tt * TT:(tt + 1) * TT], in_=ps[:, 0:TT], func=AF.Copy),
                                           reads=[pr], writes=[("qxT", fi, tt)])
                            for xh in range(XH):
                                for tt in range(TRx // TT):
                                    pis = []
                                    for mt in range(ML // 128):
                                        ps, pr = nps()
                                        mm_group(ps[:, 0:TT], pr, [(kxT[:, xh * 2 + dt, mt * 128:(mt + 1) * 128], qxT[:, xh * 2 + dt, tt * TT:(tt + 1) * TT]) for dt in range(2)],
                                                 [("qxT", xh * 2 + dt, tt) for dt in range(2)])
                                        pi = pxc[0] % 4
                                        pxc[0] += 1
                                        op("scalar", lambda e: e.activation(out=Px[pi][:], in_=ps[:, 0:TT], func=AF.Exp, scale=xsc), reads=[pr], writes=["Px%d" % pi])
                                        pis.append(pi)
                                    psd, prd = nps()
                                    mm_group(psd[:, 0:TT], prd, [(ones[:], Px[pi][:]) for pi in pis], ["Px%d" % pi for pi in pis])
                                    ri = (xh * (TRx // TT) + tt) % 2
                                    op("vector", lambda e: e.reciprocal(out=rcx[ri][:], in_=psd[:, 0:TT]), reads=[prd], writes=["rcx%d" % ri])
                                    for dvt in range(2):
                                        pso, pro = nps()
                                        mm_group(pso[:, 0:TT], pro, [(vx[:, mt, xh * 256 + dvt * 128:xh * 256 + (dvt + 1) * 128], Px[pis[mt]][:]) for mt in range(ML // 128)],
                                                 ["Px%d" % pi for pi in pis])
                                        op("vector", lambda e: e.tensor_tensor(out=oxT[:, xh * 2 + dvt, tt * TT:(tt + 1) * TT], in0=pso[:, 0:TT], in1=rcx[ri][:], op=ALU.mult),
                                           reads=[pro, "rcx%d" % ri], writes=[("oxT", xh * 2 + dvt, tt)])
                            okeys = [("oxT", f, tt) for f in range(XK) for tt in range(TRx // TT)]
                            for c0 in range(0, D, WCm):
                                wt, wtr = wso.load(Wxo[0], Wxo[1], XK, c0, WCm)
                                for tc in range(TRx // 128):
                                    rows = slice(tok0 + tc * 128, tok0 + (tc + 1) * 128)
                                    ps, pr = nps()
                                    mm_group(ps[:, 0:WCm], pr, [(oxT[:, k, tc * 128:(tc + 1) * 128], wt[:, k, 0:WCm]) for k in range(XK)], okeys + [wtr])
                                    li = (c0 // WCm + tc) % 2
                                    la, lar = ld[li], "ldx%d" % li
                                    op("sync", lambda e: e.dma_start(out=la[:, 0:WCm], in_=X2[rows, c0:c0 + WCm]), writes=[lar], dma=lar)
                                    so, sor = st1[li], "stx%d" % li
                                    op("vector", lambda e: e.tensor_tensor(out=so[:, 0:WCm], in0=ps[:, 0:WCm], in1=la[:, 0:WCm], op=ALU.add),
                                       reads=[pr, lar], writes=[sor])
                                    op("scalar", lambda e: e.dma_start(out=X3[rows, c0:c0 + WCm], in_=so[:, 0:WCm]), reads=[sor], writes=[sor + "d"], dma=sor)
                kb.barrier()

            if STOP >= 7 and not hist:
                BIG = 1.0e9
                with ExitStack() as ph:
                    gmoe = load_gain(ph, g_moe, D, "gmoe")
                    brt = load_gain(ph, b_r, NR, "brt")
                    wr = sb(ph, "wr", [128, KD, NR], BF16)
                    op("sync", lambda e: e.dma_start(out=wr[:], in_=Wr[0].rearrange("(c p) n -> p c n", p=128)), writes=["wr"], dma="const")
                    op("sync", lambda e: e.nop(), reads=["wr"])
                    xnq = sb(ph, "xnq", [128, KD, TQ], BF16)
                    WT = sb(ph, "WT", [NE, TQ])
                    nt = NormT(ph, "nq")
                    names = ["lg", "mxg", "g1h", "sh", "eg", "sg1", "gtop", "pen", "le", "m1", "k1", "le2", "m2", "k2", "dm", "ed", "e1", "r1", "w1", "w2", "wa", "wf"]
                    widths = dict(lg=NR, g1h=NG, sh=NG, eg=NG, pen=NG, le=NE, k1=NE, le2=NE, k2=NE, wa=NE, wf=NE)
                    tl = {nm: sb(ph, nm, [128, widths.get(nm, 1)]) for nm in names}

                    def bc(t, n):
                        return t[:, 0:1].to_broadcast([128, n])
                    V = "vector"
                    for q in range(T // TQ):
                        tok0 = q * TQ
                        xkeys = nt.run(X3[tok0:tok0 + TQ, :], TQ // 128, gmoe, "gmoe", xnq, "xnq")
                        op("scalar", lambda e: e.dma_start(out=XNT[:, tok0:tok0 + TQ].rearrange("(c p) t -> p c t", p=128), in_=xnq[:]),
                           reads=xkeys, writes=xkeys, dma="xnq")
                        for tc in range(TQ // 128):
                            ps, pr = nps()
                            mm_group(ps[:, 0:NR], pr, [(xnq[:, k, tc * 128:(tc + 1) * 128], wr[:, k, :]) for k in range(KD)], xkeys + ["wr"])
                            lg, mxg, g1h, sh, eg, sg1, gtop, pen, le, m1 = [tl[n_] for n_ in names[:10]]
                            k1, le2, m2, k2, dm, ed, e1, r1, w1, w2, wa, wf = [tl[n_] for n_ in names[10:]]
                            op(V, lambda e: e.tensor_tensor(out=lg[:], in0=ps[:, 0:NR], in1=brt[:], op=ALU.add), reads=[pr, "brt"], writes=["lg"])
                            op(V, lambda e: e.reduce_max(out=mxg[:], in_=lg[:, 0:NG], axis=AX.X), reads=["lg"], writes=["mxg"])
                            op(V, lambda e: e.tensor_tensor(out=g1h[:], in0=lg[:, 0:NG], in1=bc(mxg, NG), op=ALU.is_equal), reads=["lg", "mxg"], writes=["g1h"])
                            op(V, lambda e: e.tensor_tensor(out=sh[:], in0=lg[:, 0:NG], in1=bc(mxg, NG), op=ALU.subtract), reads=["lg", "mxg"], writes=["sh"])
                            op("scalar", lambda e: e.activation(out=eg[:], in_=sh[:], func=AF.Exp, accum_out=sg1[:]), reads=["sh"], writes=["eg", "sg1"])
                            op(V, lambda e: e.reciprocal(out=gtop[:], in_=sg1[:]), reads=["sg1"], writes=["gtop"])
                            op(V, lambda e: e.tensor_scalar(out=pen[:], in0=g1h[:], scalar1=-1.0, scalar2=BIG, op0=ALU.add, op1=ALU.mult), reads=["g1h"], writes=["pen"])
                            op(V, lambda e: e.tensor_tensor(out=le[:].rearrange("p (g x) -> p g x", g=NG), in0=lg[:, NG:NR].rearrange("p (g x) -> p g x", g=NG),
                                                            in1=pen[:].unsqueeze(2).to_broadcast([128, NG, EPG]), op=ALU.add), reads=["lg", "pen"], writes=["le"])
                            op(V, lambda e: e.reduce_max(out=m1[:], in_=le[:], axis=AX.X), reads=["le"], writes=["m1"])
                            op(V, lambda e: e.tensor_tensor(out=k1[:], in0=le[:], in1=bc(m1, NE), op=ALU.is_equal), reads=["le", "m1"], writes=["k1"])
                            op(V, lambda e: e.scalar_tensor_tensor(out=le2[:], in0=k1[:], scalar=-BIG, in1=le[:], op0=ALU.mult, op1=ALU.add), reads=["k1", "le"], writes=["le2"])
                            op(V, lambda e: e.reduce_max(out=m2[:], in_=le2[:], axis=AX.X), reads=["le2"], writes=["m2"])
                            op(V, lambda e: e.tensor_tensor(out=k2[:], in0=le2[:], in1=bc(m2, NE), op=ALU.is_equal), reads=["le2", "m2"], writes=["k2"])
                            op(V, lambda e: e.tensor_tensor(out=dm[:], in0=m2[:], in1=m1[:], op=ALU.subtract), reads=["m1", "m2"], writes=["dm"])
                            op("scalar", lambda e: e.activation(out=ed[:], in_=dm[:], func=AF.Exp), reads=["dm"], writes=["ed"])
                            op(V, lambda e: e.tensor_scalar(out=e1[:], in0=ed[:], scalar1=1.0, scalar2=1.0, op0=ALU.add, op1=ALU.mult), reads=["ed"], writes=["e1"])
                            op(V, lambda e: e.reciprocal(out=r1[:], in_=e1[:]), reads=["e1"], writes=["r1"])
                            op(V, lambda e: e.tensor_tensor(out=w1[:], in0=r1[:], in1=gtop[:], op=ALU.mult), reads=["r1", "gtop"], writes=["w1"])
                            op(V, lambda e: e.tensor_tensor(out=w2[:], in0=w1[:], in1=ed[:], op=ALU.mult), reads=["w1", "ed"], writes=["w2"])
                            op(V, lambda e: e.tensor_tensor(out=wa[:], in0=k1[:], in1=bc(w1, NE), op=ALU.mult), reads=["k1", "w1"], writes=["wa"])
                            op(V, lambda e: e.scalar_tensor_tensor(out=wf[:], in0=k2[:], scalar=w2[:, 0:1], in1=wa[:], op0=ALU.mult, op1=ALU.add), reads=["k2", "w2", "wa"], writes=["wf"])
                            pst, prt = nps()
                            op("tensor", lambda e: e.transpose(pst[0:NE, 0:128], wf[:], idf[:]), reads=["wf", "idf"], writes=[prt])
                            op(V, lambda e: e.tensor_copy(out=WT[:, tc * 128:(tc + 1) * 128], in_=pst[0:NE, 0:128]), reads=[prt], writes=[("WT", tc)])
                        wtk = [("WT", tc) for tc in range(TQ // 128)]
                        op("sync", lambda e: e.dma_start(out=WTD[q, :, :], in_=WT[:]), reads=wtk, writes=wtk, dma="WT")
                kb.barrier()

            if STOP >= 8 and not hist:
                with ExitStack() as ph:
                    xnT = sb(ph, "xnT", [128, KD, TQ], BF16)
                    yacc = sb(ph, "yacc", [128, KD, TQ])
                    gcol = 256 if DE >= 256 else DE
                    NGH = DE // gcol
                    HPG = gcol // 128
                    wgu = [sb(ph, "wgu", [128, KD, gcol], BF16) for _ in range(3)]
                    wdt = sb(ph, "wdt", [128, HT, D], BF16)
                    sgs = sb(ph, "sgs", [128, HT, TQ])
                    hid = sb(ph, "hid", [128, HT, TQ], BF16)
                    wbt = [sb(ph, "wbt", [128, TQ]) for _ in range(2)]
                    tmpu = [sb(ph, "tmpu", [128, TQ]) for _ in range(2)]
                    gctr = [0]
                    for q in range(T // TQ):
                        tok0 = q * TQ
                        op("sync", lambda e: e.dma_start(out=xnT[:], in_=XNT[:, tok0:tok0 + TQ].rearrange("(c p) t -> p c t", p=128)), writes=["xnT"], dma="xnT")
                        for ex in range(NE):
                            rk, j = ex // EL, ex % EL
                            wi = ex % 2
                            op("sync", lambda e: e.dma_start(out=wbt[wi][:], in_=WTD[q, ex:ex + 1, :].partition_broadcast(128)), writes=["wbt%d" % wi], dma="wbt%d" % wi)
                            for which in range(2):
                                Wsrc = (Wg if which == 0 else Wu)[j]
                                if which == 1:
                                    op("sync", lambda e: e.dma_start(out=wdt[:], in_=Wd[j][0][rk * DE:(rk + 1) * DE, :].rearrange("(c p) n -> p c n", p=128)),
                                       reads=[Wd[j][1]], writes=["wdt"], dma="wdt")
                                for gh in range(NGH):
                                    gi = gctr[0] % 3
                                    gctr[0] += 1
                                    wtile, wres = wgu[gi], "wgu%d" % gi
                                    op("sync", lambda e: e.dma_start(
                                        out=wtile[:], in_=Wsrc[0][rk * D:(rk + 1) * D, gh * gcol:(gh + 1) * gcol].rearrange("(c p) n -> p c n", p=128)),
                                       reads=[Wsrc[1]], writes=[wres], dma=wres)
                                    for hl in range(HPG):
                                        ht = gh * HPG + hl
                                        ps, pr = nps()
                                        mm_group(ps[:, 0:TQ], pr, [(wtile[:, k, hl * 128:(hl + 1) * 128], xnT[:, k, :]) for k in range(KD)], ["xnT", wres])
                                        if which == 0:
                                            op("scalar", lambda e: e.activation(out=sgs[:, ht, :], in_=ps[:, 0:TQ], func=AF.Silu), reads=[pr], writes=[("sgs", ht)])
                                        else:
                                            ti = ht % 2
                                            op("vector", lambda e: e.tensor_tensor(out=tmpu[ti][:], in0=sgs[:, ht, :], in1=ps[:, 0:TQ], op=ALU.mult),
                                               reads=[pr, ("sgs", ht)], writes=["tmpu%d" % ti])
                                            op("gpsimd", lambda e: e.tensor_tensor(out=hid[:, ht, :], in0=tmpu[ti][:], in1=wbt[wi][:], op=ALU.mult),
                                               reads=["tmpu%d" % ti, "wbt%d" % wi], writes=[("hid", ht)])
                            for f in range(KD):
                                ps, pr = nps()
                                mm_group(ps[:, 0:TQ], pr, [(wdt[:, ht, f * 128:(f + 1) * 128], hid[:, ht, :]) for ht in range(HT)], [("hid", ht) for ht in range(HT)] + ["wdt"])
                                if ex == 0:
                                    op("vector", lambda e: e.tensor_copy(out=yacc[:, f, :], in_=ps[:, 0:TQ]), reads=[pr], writes=[("yacc", f)])
                                else:
                                    op("vector", lambda e: e.tensor_tensor(out=yacc[:, f, :], in0=yacc[:, f, :], in1=ps[:, 0:TQ], op=ALU.add),
                                       reads=[pr, ("yacc", f)], writes=[("yacc", f)])
                        yk = [("yacc", f) for f in range(KD)]
                        op("scalar", lambda e: e.dma_start(out=YT[:, tok0:tok0 + TQ].rearrange("(c p) t -> p c t", p=128), in_=yacc[:]), reads=yk, writes=yk, dma="yacc")
                kb.barrier()

            if STOP >= 9 and not hist:
                with ExitStack() as ph:
                    gfin = load_gain(ph, g_fin, D, "gfin")
                    ytl = [sb(ph, "ytl", [128, KD, 128]) for _ in range(2)]
                    xo = [sb(ph, "xo", [128, D]) for _ in range(2)]
                    xs = [sb(ph, "xs", [128, D]) for _ in range(2)]
                    jb = sb(ph, "jb", [128, D], BF16)
                    yo = [sb(ph, "yo", [128, D]) for _ in range(2)]
                    fss = sb(ph, "fss", [128, 1]); fms = sb(ph, "fms", [128, 1]); fsd = sb(ph, "fsd", [128, 1]); frs = sb(ph, "frs", [128, 1])
                    for n in range(NCH):
                        b = n % 2
                        rows = slice(n * 128, (n + 1) * 128)
                        op("sync", lambda e: e.dma_start(out=xo[b][:], in_=X3[rows, :]), writes=["xo%d" % b], dma="xo%d" % b)
                        op("sync", lambda e: e.dma_start(out=ytl[b][:], in_=YT[:, n * 128:(n + 1) * 128].rearrange("(c p) t -> p c t", p=128)), writes=["ytl%d" % b], dma="ytl%d" % b)
                        for f4 in range(0, KD, 4):
                            nk = min(4, KD - f4)
                            ps, pr = nps()

                            def tr4(e):
                                for jj in range(nk):
                                    i = e.transpose(ps[:, jj * 128:(jj + 1) * 128], ytl[b][:, f4 + jj, :], idf[:])
                                return i
                            op("tensor", tr4, reads=["ytl%d" % b, "idf"], writes=[pr])
                            op("vector", lambda e: e.tensor_tensor(out=xs[b][:, f4 * 128:(f4 + nk) * 128], in0=xo[b][:, f4 * 128:(f4 + nk) * 128], in1=ps[:, 0:nk * 128], op=ALU.add),
                               reads=[pr, "xo%d" % b], writes=[("xs%d" % b, f4)])
                        xsk = [("xs%d" % b, f4) for f4 in range(0, KD, 4)]
                        op("scalar", lambda e: e.activation(out=jb[:], in_=xs[b][:], func=AF.Square, accum_out=fss[:]), reads=xsk, writes=["jb", "fss"])
                        op("vector", lambda e: e.tensor_scalar(out=fms[:], in0=fss[:], scalar1=1.0 / D, scalar2=RMS_EPS, op0=ALU.mult, op1=ALU.add), reads=["fss"], writes=["fms"])
                        op("scalar", lambda e: e.activation(out=fsd[:], in_=fms[:], func=AF.Sqrt), reads=["fms"], writes=["fsd"])
                        op("vector", lambda e: e.reciprocal(out=frs[:], in_=fsd[:]), reads=["fsd"], writes=["frs"])
                        op("vector", lambda e: e.scalar_tensor_tensor(out=yo[b][:], in0=xs[b][:], scalar=frs[:, 0:1], in1=gfin[:], op0=ALU.mult, op1=ALU.mult),
                           reads=xsk + ["frs", "gfin"], writes=["yo%d" % b])
                        op("sync", lambda e: e.dma_start(out=y_out[rows, :], in_=yo[b][:]), reads=["yo%d" % b], writes=["yo%d" % b], dma="yo%d" % b)
                kb.barrier()
        print("bass ops:", kb.nops, "sems:", len(kb.sems), "max sem value:", max(kb.semval.values()))
    return nc


def host_inputs(c, inp):
    f32 = np.float32
    bf = ml_dtypes.bfloat16
    D, T, H, NCH = c.D, c.T, c.H, c.NCH
    x = np.asarray(inp["x"], f32)
    mem = np.asarray(inp["mem"], f32)
    w_in = np.asarray(inp["w_in"], f32)[0]
    w_r = np.concatenate([np.asarray(inp["w_router_group"], f32)[0], np.asarray(inp["w_router_expert"], f32)[0]], axis=1)
    b_r = np.concatenate([np.asarray(inp["b_router_group"], f32)[0], np.asarray(inp["b_router_expert"], f32)[0]])[None, :]
    wg = np.asarray(inp["w_exp_gate"], f32)[0].reshape(c.NE * D, c.DE)
    wu = np.asarray(inp["w_exp_up"], f32)[0].reshape(c.NE * D, c.DE)
    wd = np.asarray(inp["w_exp_down"], f32)[0].reshape(c.NE * c.DE, D)
    hh = np.arange(H, dtype=f32)
    log_gamma = np.log1p(-np.exp2(-5.0 - hh)).astype(f32)
    idx = np.arange(128, dtype=f32)
    diff = idx[None, :] - idx[:, None]
    dec = np.where(diff[:, None, :] >= 0, np.exp(log_gamma[None, :, None] * np.maximum(diff, 0.0)[:, None, :]), 0.0).astype(f32)
    qkd = np.zeros((128, 3, H), f32)
    qkd[:, 0, :] = np.exp(log_gamma[None, :] * (idx + 1.0)[:, None])
    qkd[:, 1, :] = np.exp(log_gamma[None, :] * (128 - 1.0 - idx)[:, None])
    qkd[:, 2, :] = np.exp(log_gamma * 128.0)[None, :]
    half = 64
    inv = (ROPE_BASE ** (-np.arange(half, dtype=f32) / half)).astype(f32)
    qi = np.arange(128)[None, :]
    kj = np.arange(128)[:, None]
    m_prev = (kj > qi).astype(f32)
    m_cur = (kj <= qi).astype(f32)
    idb = np.eye(128, dtype=f32).astype(bf)
    idf = np.eye(128, dtype=f32)
    ones = np.ones((128, 128), f32).astype(bf)
    maps = []
    NCORES, NSEG, NW = c.NC, c.NS, c.NW
    for core in range(NCORES):
        b, s = core // NSEG, core % NSEG
        t0 = s * T
        NSG = c.NSG
        TS = T * NSG
        t0 = s * TS
        pos = (t0 + np.arange(TS)).astype(f32)
        ang = pos[:, None] * inv[None, :]
        cosv, sinv = np.cos(ang).astype(f32), np.sin(ang).astype(f32)
        ks = f32(128.0 ** -0.5)
        cs = np.stack([cosv, sinv, cosv * ks, sinv * ks], axis=1)
        cs = cs.reshape(NSG, NCH, 128, 4, 64).transpose(0, 2, 1, 3, 4).copy()
        coef = np.zeros((128, 8, H), f32)
        for j in range(NCORES if NSEG > 1 else 0):
            bj, sj = j // NSEG, j % NSEG
            if bj == b and sj < s:
                coef[:, j, :] = np.exp(log_gamma * f32(TS * (s - 1 - sj)))[None, :]
        mp0 = m_prev if s > 0 else np.zeros_like(m_prev)
        msk = np.stack([np.tile(m_prev, (1, 4)), np.tile(m_cur, (1, 4)), np.tile(mp0, (1, 4))], axis=1).astype(bf)
        NHT = max(c.NHS, 1) * T
        xhist = np.zeros((NHT, D), f32)
        if c.NHS > 0 and s > 0:
            xhist[NHT - t0:] = x[b, 0:t0]
        posh = ((t0 - NHT) + np.arange(NHT)).astype(f32)
        angh = posh[:, None] * inv[None, :]
        ch, sh_ = np.cos(angh).astype(f32), np.sin(angh).astype(f32)
        cs_h = np.stack([ch, sh_, ch * ks, sh_ * ks], axis=1).reshape(max(c.NHS, 1), NCH, 128, 4, 64).transpose(0, 2, 1, 3, 4).copy()
        xh = np.zeros((NSG, 128, D), f32)
        for sg_ in range(NSG):
            if t0 + sg_ * T > 0:
                xh[sg_] = x[b, t0 + sg_ * T - 128:t0 + sg_ * T]
        r = core if NW > 1 else 0
        m = {
            "x": x[b, t0:t0 + TS], "xh": xh, "xhist": xhist, "cs_h": cs_h, "mem": mem[b],
            "g_mix": np.asarray(inp["mix_norm_g"], f32)[0][None, :], "g_xat": np.asarray(inp["xattn_norm_g"], f32)[0][None, :],
            "g_mem": np.asarray(inp["mem_norm_g"], f32)[0][None, :], "g_moe": np.asarray(inp["moe_norm_g"], f32)[0][None, :],
            "g_fin": np.asarray(inp["final_norm_g"], f32)[None, :], "g_ret": np.asarray(inp["ret_norm_g"], f32)[0].reshape(1, -1),
            "sinks": np.asarray(inp["swa_sinks"], f32)[0][None, :], "b_r": b_r,
            "w_in": w_in[r * (D // NW):(r + 1) * (D // NW)],
            "w_ro": np.asarray(inp["w_ret_o"], f32)[0][r * (c.RV // NW):(r + 1) * (c.RV // NW)],
            "w_so": np.asarray(inp["w_swa_o"], f32)[0][r * (c.SQ // NW):(r + 1) * (c.SQ // NW)],
            "w_mo": np.asarray(inp["w_mix_o"], f32)[0][r * (D // NW):(r + 1) * (D // NW)],
            "w_xq": np.asarray(inp["w_xq"], f32)[0][r * (D // NW):(r + 1) * (D // NW)],
            "w_xkv": np.asarray(inp["w_xkv"], f32)[0][r * (D // NW):(r + 1) * (D // NW)],
            "w_xo": np.asarray(inp["w_xo"], f32)[0][r * (c.XW // NW):(r + 1) * (c.XW // NW)],
            "w_r": w_r[r * (D // NW):(r + 1) * (D // NW)],
            "w_g": wg[r * c.EL * D:(r + 1) * c.EL * D], "w_u": wu[r * c.EL * D:(r + 1) * c.EL * D],
            "w_d": wd[r * c.EL * c.DE:(r + 1) * c.EL * c.DE],
            "cs": cs, "dec": dec, "qkd": qkd, "coef": coef, "msk": msk, "idb": idb, "idf": idf, "ones": ones,
        }
        import os
        if os.environ.get('KNOAG'):
            full = {"in": w_in, "ro": np.asarray(inp["w_ret_o"], f32)[0], "so": np.asarray(inp["w_swa_o"], f32)[0], "mo": np.asarray(inp["w_mix_o"], f32)[0],
                    "xkv": np.asarray(inp["w_xkv"], f32)[0], "xq": np.asarray(inp["w_xq"], f32)[0], "xo": np.asarray(inp["w_xo"], f32)[0], "r": w_r}
            for nm_, wfull in full.items():
                if nm_ == "in":
                    for ci, c0 in enumerate(range(0, c.INW, c.CW)):
                        m["dbgw_in_%d" % ci] = wfull[:, c0:c0 + c.CW].astype(bf)
                else:
                    m["dbgw_%s_0" % nm_] = wfull.astype(bf)
            for nm_, wfull, rr in (("g", wg, D), ("u", wu, D), ("d", wd, c.DE)):
                w4 = wfull.reshape(NW, c.EL, rr, -1)
                for j in range(c.EL):
                    m["dbgw_%s_%d" % (nm_, j)] = w4[:, j].reshape(NW * rr, -1).astype(bf)
            for kk in ["w_in", "w_ro", "w_so", "w_mo", "w_xq", "w_xkv", "w_xo", "w_r", "w_g", "w_u", "w_d"]:
                pass
        maps.append({k: np.ascontiguousarray(v) for k, v in m.items()})
    return maps


def run(c, inp):
    nc = build(c)
    maps = host_inputs(c, inp)
    NCORES, NSEG = c.NC, c.NS
    res = run_bass_kernel_spmd(nc, maps, core_ids=list(range(NCORES)))
    out = np.zeros((c.B, c.SEQ, c.D), np.float32)
    TS = c.T * c.NSG
    for core in range(NCORES):
        b, s = core // NSEG, core % NSEG
        out[b, s * TS:(s + 1) * TS] = res.results[core]["y"]
    return out


def kernel(**inputs):
    return run(Cfg(**FULL), inputs)
```
